# Optimizing a Trainium2 kernel written in Bass

```python
import math
import jax, jax.numpy as jnp
from jax import lax
import numpy as np

D_MODEL = 2048
BATCH = 2
SEQ = 4096
DEPTH = 2

GRID_W = 64
CTX_LEN = 256
EPS = 1e-6

D_MIX = D_MODEL
D_POOL = D_MODEL // 4
POOL_WINDOWS = (2, 4, 8, 16)
POOL_GW = D_POOL // len(POOL_WINDOWS)
D_SSM = D_MODEL // 4
SSM_GROUP = 16
SSM_GROUPS = D_SSM // SSM_GROUP
SSM_STATE = 64
SSM_DT_MIN = 0.001
SSM_DT_MAX = 0.1
QK_NOPE = 128
QK_ROPE = 64
QK_HEAD = QK_NOPE + QK_ROPE
V_HEAD = 128
D_MLA = D_MIX - D_POOL - D_SSM
MLA_HEADS = D_MLA // V_HEAD
Q_LORA = D_MODEL // 4
KV_LORA = D_MODEL // 8
ROPE_BASE = 10000.0
Q_BLOCK = 128
OFF_POOL = 0
OFF_Q = OFF_POOL + D_POOL
OFF_SSM = OFF_Q + Q_LORA
OFF_KV = OFF_SSM + D_SSM
OFF_KR = OFF_KV + KV_LORA
IN_W = OFF_KR + QK_ROPE
N_EXPERTS = 32
TOP_K = 4
D_EXPERT = D_MODEL
SWIGLU_ALPHA = 1.702
SWIGLU_LIMIT = 7.0
EXPERT_BLOCK = 256

kernel_name = "hybrid_pool_s5_mla_moe_diffusion_trunk"


def rms_norm(x, g):
    xf = x.astype(jnp.float32)
    y = xf * lax.rsqrt(jnp.mean(jnp.square(xf), axis=-1, keepdims=True) + EPS)
    return (y * g.astype(jnp.float32)).astype(x.dtype)


def ada_chunks(cvec, w_ada, b_ada, n_chunks):
    m = jax.nn.silu(cvec) @ w_ada[:, :n_chunks * D_MODEL] + b_ada[:n_chunks * D_MODEL]
    return jnp.split(m, n_chunks, axis=-1)


def axial_rope_tables(n_tokens):
    rows = n_tokens // GRID_W
    row = jnp.broadcast_to(jnp.arange(rows)[:, None], (rows, GRID_W)).reshape(-1).astype(jnp.float32)
    col = jnp.broadcast_to(jnp.arange(GRID_W)[None, :], (rows, GRID_W)).reshape(-1).astype(jnp.float32)
    half = QK_ROPE // 2
    inv_freq = ROPE_BASE ** (-jnp.arange(0, half, 2, dtype=jnp.float32) / half)
    ang = jnp.concatenate([row[:, None] * inv_freq, col[:, None] * inv_freq], axis=-1)
    return jnp.cos(ang), jnp.sin(ang)


def apply_rope(t, cos, sin):
    half = QK_ROPE // 2
    tf = t.astype(jnp.float32)
    t1, t2 = tf[..., :half], tf[..., half:]
    cc, ss = cos[None, :, None, :], sin[None, :, None, :]
    return jnp.concatenate([t1 * cc - t2 * ss, t1 * ss + t2 * cc], axis=-1).astype(t.dtype)


def rope_tail(t, cos, sin):
    return jnp.concatenate([t[..., :QK_NOPE], apply_rope(t[..., QK_NOPE:], cos, sin)], axis=-1)


def pool_mixer(u, w_pool, pool_scale):
    B, L, _ = u.shape
    uf = u.astype(jnp.float32)
    cs = jnp.pad(jnp.cumsum(uf, axis=1), ((0, 0), (1, 0), (0, 0)))
    t = jnp.arange(L)
    parts = []
    for gi, w in enumerate(POOL_WINDOWS):
        lo = jnp.clip(t - w // 2, 0, L)
        hi = jnp.clip(t + (w - w // 2), 0, L)
        cs_g = cs[..., gi * POOL_GW:(gi + 1) * POOL_GW]
        mean = (cs_g[:, hi] - cs_g[:, lo]) / (hi - lo).astype(jnp.float32)[None, :, None]
        parts.append(mean - uf[..., gi * POOL_GW:(gi + 1) * POOL_GW])
    d = jnp.stack(parts, axis=2).astype(u.dtype)
    y = jnp.einsum('blgc,gcd->blgd', d, w_pool).reshape(B, L, D_POOL)
    return y * pool_scale


def s5_discretise(a_re, a_im, log_step):
    step = jnp.exp(log_step.astype(jnp.float32))[:, None]
    ar, ai = a_re.astype(jnp.float32), a_im.astype(jnp.float32)
    mag = jnp.exp(ar * step)
    ab_re, ab_im = mag * jnp.cos(ai * step), mag * jnp.sin(ai * step)
    den = ar * ar + ai * ai
    nr, ni = ab_re - 1.0, ab_im
    coef_re = (nr * ar + ni * ai) / den
    coef_im = (ni * ar - nr * ai) / den
    return ab_re, ab_im, coef_re, coef_im


def _complex_linear_combine(e1, e2):
    a1r, a1i, b1r, b1i = e1
    a2r, a2i, b2r, b2i = e2
    return (a2r * a1r - a2i * a1i, a2r * a1i + a2i * a1r,
            a2r * b1r - a2i * b1i + b2r, a2r * b1i + a2i * b1r + b2i)


def s5_states(u, disc, b_re, b_im, h0=None):
    ab_re, ab_im, coef_re, coef_im = disc
    bu_re = jnp.einsum('blgc,gpc->blgp', u, b_re.astype(jnp.float32))
    bu_im = jnp.einsum('blgc,gpc->blgp', u, b_im.astype(jnp.float32))
    x_re = coef_re * bu_re - coef_im * bu_im
    x_im = coef_re * bu_im + coef_im * bu_re
    a_re = jnp.broadcast_to(ab_re, x_re.shape)
    a_im = jnp.broadcast_to(ab_im, x_re.shape)
    pr, pi, hr, hi = lax.associative_scan(_complex_linear_combine, (a_re, a_im, x_re, x_im), axis=1)
    if h0 is not None:
        h0r, h0i = h0[0][:, None], h0[1][:, None]
        hr = hr + pr * h0r - pi * h0i
        hi = hi + pr * h0i + pi * h0r
    return hr, hi


def s5_readout(hr, hi, c_re, c_im):
    return (jnp.einsum('blgp,gcp->blgc', hr, c_re.astype(jnp.float32))
            - jnp.einsum('blgp,gcp->blgc', hi, c_im.astype(jnp.float32)))


def _flip(z, direction):
    return z if direction == 0 else jnp.flip(z, axis=1)


def bidirectional_s5(u_lat, u_ctx, a_re, a_im, log_step, b_re, b_im, c_re, c_im, ctx_out):
    B, L, _ = u_lat.shape
    Lc = u_ctx.shape[1]
    ul = u_lat.astype(jnp.float32).reshape(B, L, SSM_GROUPS, SSM_GROUP)
    uc = u_ctx.astype(jnp.float32).reshape(B, Lc, SSM_GROUPS, SSM_GROUP)
    y_lat, y_ctx = 0.0, 0.0
    for d in range(2):
        disc = s5_discretise(a_re[d], a_im[d], log_step[d])
        hc_r, hc_i = s5_states(_flip(uc, d), disc, b_re[d], b_im[d])
        hl_r, hl_i = s5_states(_flip(ul, d), disc, b_re[d], b_im[d], h0=(hc_r[:, -1], hc_i[:, -1]))
        y_lat = y_lat + _flip(s5_readout(hl_r, hl_i, c_re[d], c_im[d]), d)
        if ctx_out:
            y_ctx = y_ctx + _flip(s5_readout(hc_r, hc_i, c_re[d], c_im[d]), d)
    return y_lat, (y_ctx if ctx_out else None)


def s5_output(y, u, d_skip, w_glu, b_glu):
    B, L, _ = u.shape
    z = y.reshape(B, L, D_SSM) + d_skip.astype(jnp.float32) * u.astype(jnp.float32)
    g = jax.nn.gelu(z).astype(u.dtype)
    return g * jax.nn.sigmoid(g @ w_glu + b_glu)


def mla_queries(c_q, q_a_norm, w_uq, q_norm):
    B, L, _ = c_q.shape
    q = (rms_norm(c_q, q_a_norm) @ w_uq).reshape(B, L, MLA_HEADS, QK_HEAD)
    return rms_norm(q, q_norm)


def mla_keys_values(c_kv, k_r, kv_a_norm, w_ukv, k_norm):
    B, L, _ = c_kv.shape
    kv = (rms_norm(c_kv, kv_a_norm) @ w_ukv).reshape(B, L, MLA_HEADS, QK_NOPE + V_HEAD)
    k_nope, v = kv[..., :QK_NOPE], kv[..., QK_NOPE:]
    k = jnp.concatenate([k_nope, jnp.broadcast_to(k_r[:, :, None, :], (B, L, MLA_HEADS, QK_ROPE))], axis=-1)
    return rms_norm(k, k_norm), v


def attend(q, k, v):
    s = jnp.einsum('bqhd,bkhd->bhqk', q, k, preferred_element_type=jnp.float32) * (QK_HEAD ** -0.5)
    p = jax.nn.softmax(s, axis=-1).astype(v.dtype)
    return jnp.einsum('bhqk,bkhd->bqhd', p, v)


def blocked_attend(q, k, v):
    B, L, H, dk = q.shape
    nb = L // Q_BLOCK
    qb = q.reshape(B, nb, Q_BLOCK, H, dk).transpose(1, 0, 2, 3, 4)
    out = lax.map(lambda blk: attend(blk, k, v), qb)
    return out.transpose(1, 0, 2, 3, 4).reshape(B, L, H * V_HEAD)


def merge_groups(y_pool, y_ssm, y_mla, out_norm, w_out):
    parts = [rms_norm(y_pool, out_norm[:D_POOL]),
             rms_norm(y_ssm, out_norm[D_POOL:D_POOL + D_SSM]),
             rms_norm(y_mla, out_norm[D_POOL + D_SSM:])]
    return jnp.concatenate(parts, axis=-1) @ w_out


def moe(h, w_router, b_router, w_gate, b_gate, w_up, b_up, w_down, b_down):
    T, D = h.shape
    logits = (h @ w_router + b_router).astype(jnp.float32)
    top_val, top_idx = lax.top_k(logits, TOP_K)
    gates = jax.nn.softmax(top_val, axis=-1)
    n = T * TOP_K
    flat_e = top_idx.reshape(-1).astype(jnp.int32)
    flat_tok = (jnp.arange(n) // TOP_K).astype(jnp.int32)
    flat_g = gates.reshape(-1)
    order = jnp.argsort(flat_e)
    se = flat_e[order]
    counts = jnp.bincount(flat_e, length=N_EXPERTS)
    starts = jnp.cumsum(counts) - counts
    padded = ((counts + EXPERT_BLOCK - 1) // EXPERT_BLOCK) * EXPERT_BLOCK
    pad_ends = jnp.cumsum(padded)
    pad_starts = pad_ends - padded
    dest = pad_starts[se] + jnp.arange(n) - starts[se]
    n_blocks = -(-n // EXPERT_BLOCK) + N_EXPERTS
    cap = n_blocks * EXPERT_BLOCK
    row_tok = jnp.full((cap,), T, jnp.int32).at[dest].set(flat_tok[order])
    row_gate = jnp.zeros((cap,), jnp.float32).at[dest].set(flat_g[order])
    block_e = jnp.minimum(jnp.searchsorted(pad_ends, jnp.arange(n_blocks) * EXPERT_BLOCK, side='right'),
                          N_EXPERTS - 1)
    h_pad = jnp.concatenate([h, jnp.zeros((1, D), h.dtype)], axis=0)

    def expert_block(args):
        tok, e = args
        xb = h_pad[tok]
        g = jnp.minimum(xb @ w_gate[e] + b_gate[e], SWIGLU_LIMIT)
        u = jnp.clip(xb @ w_up[e] + b_up[e], -SWIGLU_LIMIT, SWIGLU_LIMIT)
        act = g * jax.nn.sigmoid(SWIGLU_ALPHA * g) * (u + 1.0)
        return act @ w_down[e] + b_down[e]

    out = lax.map(expert_block, (row_tok.reshape(n_blocks, EXPERT_BLOCK), block_e))
    out = out.reshape(cap, D).astype(jnp.float32) * row_gate[:, None]
    y = jnp.zeros((T + 1, D), jnp.float32).at[row_tok].add(out)[:T]
    return y.astype(h.dtype)


def hybrid_layer(x, ctx, c, c_ctx, cos, sin, prm, ctx_out):
    (w_ada, b_ada, norm_mix, norm_ffn, w_in, w_pool, pool_scale,
     ssm_a_re, ssm_a_im, ssm_log_step, ssm_b_re, ssm_b_im, ssm_c_re, ssm_c_im,
     ssm_d, w_glu, b_glu, q_a_norm, w_uq, kv_a_norm, w_ukv, q_norm, k_norm,
     out_norm, w_out, w_router, b_router, w_gate, b_gate, w_up, b_up, w_down, b_down) = prm
    B, L, D = x.shape
    Lc = ctx.shape[1]
    sh1, sc1, g1, sh2, sc2, g2 = [m[:, None, :] for m in ada_chunks(c, w_ada, b_ada, 6)]
    cmod = ada_chunks(c_ctx, w_ada, b_ada, 6 if ctx_out else 2)

    h = rms_norm(x, norm_mix) * (1.0 + sc1) + sh1
    hc = rms_norm(ctx, norm_mix) * (1.0 + cmod[1]) + cmod[0]
    proj = h @ w_in
    c0 = OFF_POOL if ctx_out else OFF_SSM
    proj_c = hc @ w_in[:, c0:]
    pc = lambda a, b: proj_c[..., a - c0:b - c0]

    y_pool = pool_mixer(proj[..., OFF_POOL:OFF_Q], w_pool, pool_scale)
    u_ssm, u_ssm_c = proj[..., OFF_SSM:OFF_KV], pc(OFF_SSM, OFF_KV)
    ys_lat, ys_ctx = bidirectional_s5(u_ssm, u_ssm_c, ssm_a_re, ssm_a_im, ssm_log_step,
                                      ssm_b_re, ssm_b_im, ssm_c_re, ssm_c_im, ctx_out)
    y_ssm = s5_output(ys_lat, u_ssm, ssm_d, w_glu, b_glu)
    q = rope_tail(mla_queries(proj[..., OFF_Q:OFF_SSM], q_a_norm, w_uq, q_norm), cos, sin)
    k, v = mla_keys_values(proj[..., OFF_KV:OFF_KR], proj[..., OFF_KR:], kv_a_norm, w_ukv, k_norm)
    k = rope_tail(k, cos, sin)
    k_c, v_c = mla_keys_values(pc(OFF_KV, OFF_KR), pc(OFF_KR, IN_W), kv_a_norm, w_ukv, k_norm)
    y_mla = blocked_attend(q, jnp.concatenate([k_c, k], axis=1), jnp.concatenate([v_c, v], axis=1))
    x = x + g1 * merge_groups(y_pool, y_ssm, y_mla, out_norm, w_out)

    if ctx_out:
        y_pool_c = pool_mixer(pc(OFF_POOL, OFF_Q), w_pool, pool_scale)
        y_ssm_c = s5_output(ys_ctx, u_ssm_c, ssm_d, w_glu, b_glu)
        q_c = mla_queries(pc(OFF_Q, OFF_SSM), q_a_norm, w_uq, q_norm)
        y_mla_c = attend(q_c, k_c, v_c).reshape(B, Lc, D_MLA)
        ctx = ctx + cmod[2] * merge_groups(y_pool_c, y_ssm_c, y_mla_c, out_norm, w_out)

    h2 = rms_norm(x, norm_ffn) * (1.0 + sc2) + sh2
    tokens = h2.reshape(B * L, D)
    if ctx_out:
        h2c = rms_norm(ctx, norm_ffn) * (1.0 + cmod[4]) + cmod[3]
        tokens = jnp.concatenate([tokens, h2c.reshape(B * Lc, D)], axis=0)
    y = moe(tokens, w_router, b_router, w_gate, b_gate, w_up, b_up, w_down, b_down)
    x = x + g2 * y[:B * L].reshape(B, L, D)
    if ctx_out:
        ctx = ctx + cmod[5] * y[B * L:].reshape(B, Lc, D)
    return x, ctx


def setup_inputs(seed: int = 0) -> dict:
    key = jax.random.key(seed)
    keys = jax.random.split(key, 40)

    def nrm(i, shape, scale):
        return scale * jax.random.normal(keys[i], shape, jnp.float32)

    L, G, P, C, E, F, D = DEPTH, SSM_GROUPS, SSM_STATE, SSM_GROUP, N_EXPERTS, D_EXPERT, D_MODEL
    a_im_init = jnp.pi * jnp.arange(P, dtype=jnp.float32)
    return {
        "x": nrm(0, (BATCH, SEQ, D), 1.0),
        "c": nrm(1, (BATCH, D), 1.0),
        "ctx": nrm(2, (BATCH, CTX_LEN, D), 1.0),
        "c_ctx": nrm(3, (D,), 1.0),
        "w_ada": nrm(4, (L, D, 6 * D), 0.5 * D ** -0.5),
        "b_ada": nrm(5, (L, 6 * D), 0.02),
        "norm_mix": 1.0 + nrm(6, (L, D), 0.05),
        "norm_ffn": 1.0 + nrm(7, (L, D), 0.05),
        "w_in": nrm(8, (L, D, IN_W), D ** -0.5),
        "w_pool": nrm(9, (L, len(POOL_WINDOWS), POOL_GW, POOL_GW), POOL_GW ** -0.5),
        "pool_scale": 1.0 + nrm(10, (L, D_POOL), 0.1),
        "ssm_a_re": -0.5 + nrm(11, (L, 2, G, P), 0.01),
        "ssm_a_im": a_im_init + nrm(12, (L, 2, G, P), 0.01),
        "ssm_log_step": jax.random.uniform(keys[13], (L, 2, G), jnp.float32,
                                           math.log(SSM_DT_MIN), math.log(SSM_DT_MAX)),
        "ssm_b_re": nrm(14, (L, 2, G, P, C), (2 * C) ** -0.5),
        "ssm_b_im": nrm(15, (L, 2, G, P, C), (2 * C) ** -0.5),
        "ssm_c_re": nrm(16, (L, 2, G, C, P), P ** -0.5),
        "ssm_c_im": nrm(17, (L, 2, G, C, P), P ** -0.5),
        "ssm_d": nrm(18, (L, D_SSM), 1.0),
        "w_glu": nrm(19, (L, D_SSM, D_SSM), D_SSM ** -0.5),
        "b_glu": nrm(20, (L, D_SSM), 0.01),
        "q_a_norm": 1.0 + nrm(21, (L, Q_LORA), 0.05),
        "w_uq": nrm(22, (L, Q_LORA, MLA_HEADS * QK_HEAD), Q_LORA ** -0.5),
        "kv_a_norm": 1.0 + nrm(23, (L, KV_LORA), 0.05),
        "w_ukv": nrm(24, (L, KV_LORA, MLA_HEADS * (QK_NOPE + V_HEAD)), KV_LORA ** -0.5),
        "q_norm": 1.0 + nrm(25, (L, QK_HEAD), 0.05),
        "k_norm": 1.0 + nrm(26, (L, QK_HEAD), 0.05),
        "out_norm": 1.0 + nrm(27, (L, D_MIX), 0.05),
        "w_out": nrm(28, (L, D_MIX, D), D_MIX ** -0.5),
        "w_router": nrm(29, (L, D, E), D ** -0.5),
        "b_router": nrm(30, (L, E), 0.01),
        "w_gate": nrm(31, (L, E, D, F), D ** -0.5),
        "b_gate": nrm(32, (L, E, F), 0.01),
        "w_up": nrm(33, (L, E, D, F), D ** -0.5),
        "b_up": nrm(34, (L, E, F), 0.01),
        "w_down": nrm(35, (L, E, F, D), F ** -0.5),
        "b_down": nrm(36, (L, E, D), 0.01),
    }


def reference(x, c, ctx, c_ctx, w_ada, b_ada, norm_mix, norm_ffn, w_in, w_pool, pool_scale,
              ssm_a_re, ssm_a_im, ssm_log_step, ssm_b_re, ssm_b_im, ssm_c_re, ssm_c_im,
              ssm_d, w_glu, b_glu, q_a_norm, w_uq, kv_a_norm, w_ukv, q_norm, k_norm,
              out_norm, w_out, w_router, b_router, w_gate, b_gate, w_up, b_up, w_down, b_down):
    cos, sin = axial_rope_tables(x.shape[1])
    params = (w_ada, b_ada, norm_mix, norm_ffn, w_in, w_pool, pool_scale,
              ssm_a_re, ssm_a_im, ssm_log_step, ssm_b_re, ssm_b_im, ssm_c_re, ssm_c_im,
              ssm_d, w_glu, b_glu, q_a_norm, w_uq, kv_a_norm, w_ukv, q_norm, k_norm,
              out_norm, w_out, w_router, b_router, w_gate, b_gate, w_up, b_up, w_down, b_down)
    for layer in range(DEPTH):
        prm = tuple(p[layer] for p in params)
        x, ctx = hybrid_layer(x, ctx, c, c_ctx, cos, sin, prm, ctx_out=(layer < DEPTH - 1))
    return x
```

```python
from contextlib import ExitStack
import numpy as np
import concourse.bass as bass
import concourse.mybir as mybir
from concourse.bass_utils import run_bass_kernel_spmd

F32 = mybir.dt.float32
BF16 = mybir.dt.bfloat16
U32 = mybir.dt.uint32
ALU = mybir.AluOpType
AF = mybir.ActivationFunctionType
AX = mybir.AxisListType

N_CORES = 8


class Buf:
    __slots__ = ("name", "w", "r")

    def __init__(self, name=""):
        self.name = name
        self.w = None
        self.r = []


class Tile(Buf):
    __slots__ = ("t", "subs")

    def __init__(self, name, t):
        super().__init__(name)
        self.t = t
        self.subs = {}

    def __getitem__(self, idx):
        return self.t[idx]

    def sub(self, key):
        b = self.subs.get(key)
        if b is None:
            b = Buf(f"{self.name}.{key}")
            self.subs[key] = b
        return b


class Prog:
    NDSEM = 12

    def __init__(self, nc, same_engine_sync=True):
        self.nc = nc
        self.eng = {"pe": nc.tensor, "act": nc.scalar, "dve": nc.vector,
                    "pool": nc.gpsimd, "sp": nc.sync}
        self.sem = {}
        self.cnt = {}
        for e in ("pe", "act", "dve", "pool"):
            self.sem[e] = nc.alloc_semaphore(name=f"s_{e}")
            self.cnt[e] = 0
        self.dq = {}
        for q in ("sp", "act", "pool"):
            sems = [nc.alloc_semaphore(name=f"d_{q}{i}") for i in range(self.NDSEM)]
            self.dq[q] = {"sems": sems, "cnt": [0] * self.NDSEM, "next": 0}
            for i, s in enumerate(sems):
                self.sem[(q, i)] = s
        self.seen = {e: {} for e in self.eng}
        self.same_engine_sync = same_engine_sync
        self.n_instr = 0
        self.out_toks = []

    def sb(self, name, shape, dtype=F32):
        return Tile(name, self.nc.alloc_sbuf_tensor("sb_" + name, list(shape), dtype))

    def ps(self, name, shape, dtype=F32):
        return Tile(name, self.nc.alloc_psum_tensor("ps_" + name, list(shape), dtype))

    def sbx(self, es, name, shape, dtype=F32):
        return Tile(name, es.enter_context(self.nc.sbuf_tensor("sb_" + name, list(shape), dtype)))

    def psx(self, es, name, shape, dtype=F32):
        return Tile(name, es.enter_context(self.nc.psum_tensor("ps_" + name, list(shape), dtype)))

    def barrier(self):
        deps = set()
        for e in ("pe", "act", "dve", "pool"):
            if self.cnt[e] > 0:
                deps.add((e, self.cnt[e]))
        for q, dq in self.dq.items():
            for i in range(self.NDSEM):
                if dq["cnt"][i] > 0:
                    deps.add(((q, i), dq["cnt"][i]))
        for e in ("pe", "act", "dve", "pool", "sp"):
            self._wait(e, deps)

    def _deps(self, reads, writes):
        deps = set()
        for b in reads:
            if b.w is not None:
                deps.add(b.w)
        for b in writes:
            if b.w is not None:
                deps.add(b.w)
            deps.update(b.r)
        return deps

    def _wait(self, eng, deps):
        seen = self.seen[eng]
        best = {}
        for k, v in deps:
            if k == eng and (eng == "pe" or not self.same_engine_sync):
                continue
            if seen.get(k, 0) >= v:
                continue
            if best.get(k, 0) < v:
                best[k] = v
        for k, v in best.items():
            self.eng[eng].wait_ge(self.sem[k], v)
            seen[k] = v

    def _record(self, tok, reads, writes):
        for b in reads:
            b.r.append(tok)
        for b in writes:
            b.w = tok
            b.r = []

    def op(self, eng, emit, reads=(), writes=(), signal=True):
        self._wait(eng, self._deps(reads, writes))
        ins = emit(self.eng[eng])
        self.n_instr += 1
        if signal:
            self.cnt[eng] += 1
            ins.then_inc(self.sem[eng], 1)
            tok = (eng, self.cnt[eng])
        else:
            tok = (eng, self.cnt[eng] + 1)
        self._record(tok, reads, writes)
        return tok

    def dma(self, out, in_, reads=(), writes=(), q="sp", is_output=False, **kw):
        dq = self.dq[q]
        i = dq["next"]
        dq["next"] = (i + 1) % self.NDSEM
        key = (q, i)
        deps = self._deps(reads, writes)
        if dq["cnt"][i] > 0:
            deps.add((key, dq["cnt"][i]))
        self._wait(q, deps)
        ins = self.eng[q].dma_start(out=out, in_=in_, **kw)
        dq["cnt"][i] += 16
        ins.then_inc(dq["sems"][i], 16)
        tok = (key, dq["cnt"][i])
        self._record(tok, reads, writes)
        if is_output:
            self.out_toks.append(tok)
        self.n_instr += 1
        return tok

    def bound_reg(self, val):
        r = self.nc.gpsimd.alloc_register()
        self.nc.gpsimd.reg_mov(r, val)
        return r

    def idma(self, out, in_, idx_ap, scatter, bound, reads=(), writes=(), is_output=False):
        q = "pool"
        dq = self.dq[q]
        i = dq["next"]
        dq["next"] = (i + 1) % self.NDSEM
        key = (q, i)
        deps = self._deps(reads, writes)
        if dq["cnt"][i] > 0:
            deps.add((key, dq["cnt"][i]))
        self._wait(q, deps)
        off = bass.IndirectOffsetOnAxis(ap=idx_ap, axis=0)
        if scatter:
            ins = self.nc.gpsimd.indirect_dma_start(out=out, out_offset=off, in_=in_, in_offset=None,
                                                    bounds_check=bound, oob_is_err=False)
        else:
            ins = self.nc.gpsimd.indirect_dma_start(out=out, out_offset=None, in_=in_, in_offset=off,
                                                    bounds_check=bound, oob_is_err=False)
        dq["cnt"][i] += 16
        ins.then_inc(dq["sems"][i], 16)
        tok = (key, dq["cnt"][i])
        self._record(tok, reads, writes)
        if is_output:
            self.out_toks.append(tok)
        self.n_instr += 1
        return tok

    def finish(self, eng="sp"):
        self._wait(eng, set(self.out_toks))
        deps = set()
        for e in ("pe", "act", "dve", "pool"):
            if self.cnt[e] > 0:
                deps.add((e, self.cnt[e]))
        self._wait(eng, deps)

    def mm(self, out_ap, lhsT, rhs, start, stop, reads, writes):
        return self.op("pe", lambda e: e.matmul(out_ap, lhsT, rhs, start=start, stop=stop),
                       reads, writes, signal=True)

    def tr(self, out_ap, in_ap, ident_ap, reads, writes, signal=True):
        return self.op("pe", lambda e: e.transpose(out_ap, in_ap, ident_ap),
                       reads, writes, signal=True)

    def act(self, out_ap, in_ap, func, reads, writes, bias=None, scale=None, accum_out=None, eng="act"):
        kw = {}
        if bias is not None:
            kw["bias"] = bias
        if scale is not None:
            kw["scale"] = scale
        if accum_out is not None:
            kw["accum_out"] = accum_out
        return self.op("act", lambda e: e.activation(out_ap, in_ap, func, **kw), reads, writes)

    def ts(self, eng, out_ap, in0, s1, s2, op0, op1, reads, writes, accum_out=None):
        kw = {}
        if accum_out is not None:
            kw["accum_out"] = accum_out
        if op1 is None:
            return self.op(eng, lambda e: e.tensor_scalar(out_ap, in0, s1, None, op0, **kw), reads, writes)
        return self.op(eng, lambda e: e.tensor_scalar(out_ap, in0, s1, s2, op0, op1, **kw), reads, writes)

    def tt(self, eng, out_ap, in0, in1, op, reads, writes):
        return self.op(eng, lambda e: e.tensor_tensor(out_ap, in0, in1, op), reads, writes)

    def stt(self, out_ap, in0, scalar, in1, op0, op1, reads, writes):
        return self.op("dve", lambda e: e.scalar_tensor_tensor(out_ap, in0, scalar, in1, op0, op1),
                       reads, writes)

    def copy(self, eng, out_ap, in_ap, reads, writes):
        if eng == "act":
            return self.op("act", lambda e: e.copy(out_ap, in_ap), reads, writes)
        return self.op(eng, lambda e: e.tensor_copy(out_ap, in_ap), reads, writes)

    def memset(self, eng, ap, val, writes):
        return self.op(eng, lambda e: e.memset(ap, val), (), writes)


D = 2048
DEPTH = 2
B = 2
L = 4096
LC = 256
IN_W = 1856
OFF_POOL, OFF_Q, OFF_SSM, OFF_KV, OFF_KR = 0, 512, 1024, 1536, 1792
NE = 32
E_LOC = 4
CAPH = 1152
CAP = 2 * CAPH
EPS = 1e-6


def new_nc():
    return bass.Bass("TRN2", target_bir_lowering=False)


def din(nc, name, shape, dtype=F32):
    return nc.dram_tensor(name, list(shape), dtype, kind="ExternalInput").ap()


def dout(nc, name, shape, dtype=F32):
    return nc.dram_tensor(name, list(shape), dtype, kind="ExternalOutput").ap()


def dint(nc, name, shape, dtype=F32):
    return nc.dram_tensor(name, list(shape), dtype, kind="Internal").ap()


def host_consts():
    ident = np.eye(128, dtype=np.float32)
    ltri = np.triu(np.ones((128, 128), np.float32), k=1)
    return ident, ltri


def build_moe(T):
    NT = T // 128
    CT = CAPH // 128
    nc = new_nc()
    h2 = din(nc, "h2", [T, D])
    gate = din(nc, "gate", [T, E_LOC])
    wg = din(nc, "wg", [E_LOC, D, D])
    wu = din(nc, "wu", [E_LOC, D, D])
    wd = din(nc, "wd", [E_LOC, D, D])
    bg = din(nc, "bg", [E_LOC, D])
    bu = din(nc, "bu", [E_LOC, D])
    bd = din(nc, "bd", [E_LOC, D])
    identd = din(nc, "ident", [128, 128])
    ltrid = din(nc, "ltri", [128, 128])
    ypart = dout(nc, "ypart", [T, D])
    xbuf = [dint(nc, f"xbuf{e}", [CAP, D], BF16) for e in range(E_LOC)]
    ybuf = [dint(nc, f"ybuf{e}", [CAP, D], F32) for e in range(E_LOC)]
    xbuf_b = [Buf(f"xbuf{e}") for e in range(E_LOC)]
    ybuf_b = [Buf(f"ybuf{e}") for e in range(E_LOC)]

    p = Prog(nc)
    rb = p.bound_reg(CAP - 1)
    identf = p.sb("identf", [128, 128]); ltrif = p.sb("ltrif", [128, 128])
    identb = p.sb("identb", [128, 128], BF16); ltrib = p.sb("ltrib", [128, 128], BF16)
    onesb = p.sb("onesb", [128, 128], BF16)
    onesf = p.sb("onesf", [128, NT])
    G = p.sb("G", [128, NT, E_LOC])
    mask = p.sb("mask", [128, E_LOC, NT]); maskb = p.sb("maskb", [128, E_LOC, NT], BF16)
    tot = p.sb("tot", [128, E_LOC, NT]); incl = p.sb("incl", [128, E_LOC, NT])
    pos = p.sb("pos", [128, E_LOC, NT]); idx = p.sb("idx", [128, E_LOC, NT], U32)
    bgt = p.sb("bgt", [128, E_LOC, 16]); but = p.sb("but", [128, E_LOC, 16])
    bdb = p.sb("bdb", [1, D], BF16)

    p.dma(identf[:], identd, writes=[identf])
    p.dma(ltrif[:], ltrid, writes=[ltrif])
    p.dma(G[:], gate.rearrange("(n p) e -> p n e", p=128), writes=[G], allow_slow_non_contiguous=False)
    p.dma(bgt[:], bg.rearrange("e (c p) -> p e c", p=128), writes=[bgt], allow_slow_non_contiguous=True)
    p.dma(but[:], bu.rearrange("e (c p) -> p e c", p=128), writes=[but], allow_slow_non_contiguous=True)
    p.copy("dve", identb[:], identf[:], [identf], [identb])
    p.copy("dve", ltrib[:], ltrif[:], [ltrif], [ltrib])
    p.memset("dve", onesb[:], 1.0, [onesb])
    p.memset("dve", onesf[:], 1.0, [onesf])

    psA = [p.ps(f"psA{i}", [128, 512]) for i in range(2)]
    psB = [p.ps(f"psB{i}", [128, 512]) for i in range(2)]
    psY = [p.ps(f"psY{i}", [128, 512]) for i in range(2)]
    psT = [p.ps(f"psT{i}", [128, 512], BF16) for i in range(2)]

    p.ts("dve", mask[:].rearrange("p e n -> p n e"), G[:], 0.0, None, ALU.is_gt, None, [G], [mask])
    p.copy("dve", maskb[:], mask[:], [mask], [maskb])
    NF = E_LOC * NT
    mflat = maskb[:].rearrange("p e n -> p (e n)")
    p.mm(psA[0][:, :NF], ltrib[:], mflat, True, True, [ltrib, maskb], [psA[0]])
    p.mm(psB[0][:, :NF], onesb[:], mflat, True, True, [onesb, maskb], [psB[0]])
    p.copy("dve", tot[:].rearrange("p e n -> p (e n)"), psB[0][:, :NF], [psB[0]], [tot])
    for e in range(E_LOC):
        p.op("dve", lambda en, e=e: en.tensor_tensor_scan(incl[:, e, :], onesf[:], tot[:, e, :], 0.0,
                                                          ALU.mult, ALU.add), [onesf, tot], [incl])
    p.tt("dve", pos[:].rearrange("p e n -> p (e n)"), psA[0][:, :NF],
         incl[:].rearrange("p e n -> p (e n)"), ALU.add, [psA[0], incl], [pos])
    p.tt("dve", pos[:], pos[:], tot[:], ALU.subtract, [pos, tot], [pos])
    p.ts("dve", incl[:], mask[:], -1.0e6, 1.0e6, ALU.mult, ALU.add, [mask], [incl])
    p.tt("dve", pos[:], pos[:], incl[:], ALU.add, [pos, incl], [pos])
    p.copy("dve", idx[:], pos[:], [pos], [idx])

    T4 = [p.sb(f"T4_{i}", [128, D]) for i in range(4)]
    hf = T4[0:2]
    hb = [p.sb(f"hb{i}", [128, D], BF16) for i in range(2)]
    for n in range(NT):
        f = hf[n % 2]; b_ = hb[n % 2]
        p.dma(f[:], h2[n * 128:(n + 1) * 128, :], writes=[f])
        p.copy("act" if n % 2 else "dve", b_[:], f[:], [f], [b_])
        for e in range(E_LOC):
            p.idma(xbuf[e], b_[:], idx[:, e, n:n + 1], True, rb, reads=[b_, idx], writes=[xbuf_b[e]])

    XT = p.sb("XT", [128, 16, CAPH], BF16)
    AT = p.sb("AT", [128, 16, CAPH], BF16)
    NW = 4
    wq = [p.sb(f"wq{i}", [128, 16, 512], BF16) for i in range(NW)]
    wqi = [0]

    def next_w():
        t = wq[wqi[0] % NW]
        wqi[0] += 1
        return t

    xs = hb
    gsb = [p.sb(f"gsb{i}", [128, 512]) for i in range(2)]
    sgb = [p.sb(f"sgb{i}", [128, 512]) for i in range(2)]
    usb = [p.sb(f"usb{i}", [128, 512]) for i in range(2)]
    ysb = T4[2:4]
    blocks = [(s, min(512, CAPH - s)) for s in range(0, CAPH, 512)]
    cnt = 0
    for e, sb0 in [(e_, h_ * CAPH) for e_ in range(E_LOC) for h_ in range(2)]:
        p.dma(bdb[:], bd[e:e + 1, :], writes=[bdb], q="pool")
        for ct in range(CT):
            x_ = xs[ct % 2]
            p.dma(x_[:], xbuf[e][sb0 + ct * 128:sb0 + (ct + 1) * 128, :], reads=[xbuf_b[e]], writes=[x_])
            for g4 in range(4):
                pt = psT[(ct * 4 + g4) % 2]
                for j in range(4):
                    c = g4 * 4 + j
                    p.tr(pt[:, j * 128:(j + 1) * 128], x_[:, c * 128:(c + 1) * 128], identb[:],
                         [x_, identb], [pt], signal=(j == 3))
                p.copy("act" if g4 % 2 else "dve",
                       XT[:, g4 * 4:(g4 + 1) * 4, ct * 128:(ct + 1) * 128],
                       pt[:].rearrange("p (j t) -> p j t", j=4), [pt], [XT])
        for q in range(4):
            wgq = next_w(); wuq = next_w()
            p.dma(wgq[:], wg[e].rearrange("(c p) f -> p c f", p=128)[:, :, q * 512:(q + 1) * 512],
                  writes=[wgq], q="pool")
            p.dma(wuq[:], wu[e].rearrange("(c p) f -> p c f", p=128)[:, :, q * 512:(q + 1) * 512],
                  writes=[wuq], q="pool")
            for fc4 in range(4):
                fc = q * 4 + fc4
                for (s0, sn) in blocks:
                    pa = psA[cnt % 2]; pb = psB[cnt % 2]
                    g_ = gsb[cnt % 2]; s_ = sgb[cnt % 2]; u_ = usb[cnt % 2]
                    cnt += 1
                    for dc in range(16):
                        p.mm(pa[:, :sn], wgq[:, dc, fc4 * 128:(fc4 + 1) * 128], XT[:, dc, s0:s0 + sn],
                             dc == 0, dc == 15, [wgq, XT], [pa])
                    for dc in range(16):
                        p.mm(pb[:, :sn], wuq[:, dc, fc4 * 128:(fc4 + 1) * 128], XT[:, dc, s0:s0 + sn],
                             dc == 0, dc == 15, [wuq, XT], [pb])
                    p.ts("dve", g_[:, :sn], pa[:, :sn], bgt[:, e, fc:fc + 1], 7.0, ALU.add, ALU.min,
                         [pa, bgt], [g_])
                    p.act(s_[:, :sn], g_[:, :sn], AF.Sigmoid, [g_], [s_], scale=1.702)
                    p.ts("dve", u_[:, :sn], pb[:, :sn], but[:, e, fc:fc + 1], 7.0, ALU.add, ALU.min,
                         [pb, but], [u_])
                    p.ts("pool", u_[:, :sn], u_[:, :sn], -7.0, 1.0, ALU.max, ALU.add, [u_], [u_])
                    p.tt("pool", g_[:, :sn], g_[:, :sn], s_[:, :sn], ALU.mult, [g_, s_], [g_])
                    p.tt("pool", AT[:, fc, s0:s0 + sn], g_[:, :sn], u_[:, :sn], ALU.mult, [g_, u_], [AT])
        wdq = []
        for q in range(4):
            w_ = next_w()
            p.dma(w_[:], wd[e].rearrange("(c p) f -> p c f", p=128)[:, :, q * 512:(q + 1) * 512],
                  writes=[w_], q="pool")
            wdq.append(w_)
        for ct in range(CT):
            y_ = ysb[ct % 2]
            for q in range(4):
                py = psY[(ct * 4 + q) % 2]
                for fc in range(16):
                    p.mm(py[:], AT[:, fc, ct * 128:(ct + 1) * 128], wdq[q][:, fc, :],
                         fc == 0, False, [AT, wdq[q]], [py])
                p.mm(py[:], onesb[0:1, :], bdb[0:1, q * 512:(q + 1) * 512], False, True,
                     [onesb, bdb], [py])
                p.copy("act" if q % 2 else "dve", y_[:, q * 512:(q + 1) * 512], py[:], [py], [y_])
            p.dma(ybuf[e][sb0 + ct * 128:sb0 + (ct + 1) * 128, :], y_[:], reads=[y_], writes=[ybuf_b[e]])

    yg = T4[0:2]
    acc = T4[2:4]
    for t_ in yg:
        p.memset("dve", t_[:], 0.0, [t_])
    k = 0
    for n in range(NT):
        a_ = acc[n % 2]
        for e in range(E_LOC):
            y_ = yg[k % 2]; k += 1
            p.idma(y_[:], ybuf[e], idx[:, e, n:n + 1], False, rb, reads=[ybuf_b[e], idx], writes=[y_])
            if e == 0:
                p.ts("dve", a_[:], y_[:], G[:, n, e:e + 1], None, ALU.mult, None, [y_, G], [a_])
            else:
                p.stt(a_[:], y_[:], G[:, n, e:e + 1], a_[:], ALU.mult, ALU.add, [y_, G, a_], [a_])
        p.dma(ypart[n * 128:(n + 1) * 128, :], a_[:], reads=[a_], is_output=True)
    p.finish()
    return nc


ADA_COLS = 6 * D // N_CORES


def build_ada():
    nc = new_nc()
    cvT = din(nc, "cvT", [128, 16, 3])
    wa = din(nc, "wa", [DEPTH, D, ADA_COLS])
    ba = din(nc, "ba", [DEPTH, ADA_COLS])
    mods = dout(nc, "mods", [DEPTH, 3, ADA_COLS])
    p = Prog(nc)
    cv = p.sb("cv", [128, 16, 3]); sv = p.sb("sv", [128, 16, 3])
    p.dma(cv[:], cvT, writes=[cv])
    p.act(sv[:], cv[:], AF.Silu, [cv], [sv])
    w = [p.sb(f"w{i}", [128, 4, ADA_COLS]) for i in range(2)]
    ps = [p.ps(f"ps{i}", [128, 512]) for i in range(3)]
    bt = p.sb("bt", [3, ADA_COLS]); ot = p.sb("ot", [3, ADA_COLS])
    k = 0
    for l in range(DEPTH):
        for r in range(3):
            p.dma(bt[r:r + 1, :], ba[l:l + 1, :], writes=[bt])
        for c4 in range(4):
            w_ = w[k % 2]; k += 1
            p.dma(w_[:], wa[l, c4 * 512:(c4 + 1) * 512, :].rearrange("(c p) n -> p c n", p=128), writes=[w_])
            for c in range(4):
                for nb in range(3):
                    cc = c4 * 4 + c
                    p.mm(ps[nb][0:3, :], sv[:, cc, :], w_[:, c, nb * 512:(nb + 1) * 512],
                         cc == 0, cc == 15, [sv, w_], [ps[nb]])
        for nb in range(3):
            p.tt("dve", ot[:, nb * 512:(nb + 1) * 512], ps[nb][0:3, :], bt[:, nb * 512:(nb + 1) * 512],
                 ALU.add, [ps[nb], bt], [ot])
        p.dma(mods[l], ot[:], reads=[ot], is_output=True)
    p.finish()
    return nc


def tok_tiles(has_ctx):
    tiles = []
    r = 0
    if has_ctx:
        tiles.append((0, 64, True))
        r = 64
    for i in range(8):
        tiles.append((r + i * 128, 128, False))
    return tiles


def emit_rstd(p, x_t, np_, junk, ss, rs, epst, n):
    p.act(junk[:np_, :n], x_t[:np_, :n], AF.Square, [x_t], [junk, ss], accum_out=ss[:np_, :])
    p.act(rs[:np_, :], ss[:np_, :], AF.Sqrt, [ss, epst], [rs], scale=1.0 / n, bias=epst[:np_, :])
    p.op("dve", lambda e: e.reciprocal(rs[:np_, :], rs[:np_, :]), [rs], [rs])


def build_tok1(do_comb, do_proj, has_ctx=True):
    NTOK = 1088 if has_ctx else 1024
    nc = new_nc()
    xin = din(nc, "xin", [NTOK, D])
    if do_comb:
        yps = [din(nc, f"yp{k}", [NTOK, D]) for k in range(N_CORES)]
        gbl = din(nc, "gb_lat", [128, D]); gbc = din(nc, "gb_ctx", [128, D])
        xout = dout(nc, "xout", [NTOK, D])
    if do_proj:
        identd = din(nc, "ident", [128, 128])
        nm = din(nc, "nm", [128, 16])
        scl_d = din(nc, "sc_lat", [128, 16]); shl_d = din(nc, "sh_lat", [128, 16])
        scc_d = din(nc, "sc_ctx", [128, 16]); shc_d = din(nc, "sh_ctx", [128, 16])
        w_in = din(nc, "w_in", [D, IN_W])
        proj = dout(nc, "proj", [NTOK, IN_W])
    p = Prog(nc)
    xt = [p.sb(f"xt{i}", [128, D]) for i in range(2)]
    if do_comb:
        yt = [p.sb(f"yt{i}", [128, D]) for i in range(3)]
        gl = p.sb("gl", [128, D]); gc = p.sb("gc", [128, D])
        p.dma(gl[:], gbl, writes=[gl]); p.dma(gc[:], gbc, writes=[gc])
    if do_proj:
        identf = p.sb("identf", [128, 128]); identb = p.sb("identb", [128, 128], BF16)
        p.dma(identf[:], identd, writes=[identf])
        p.copy("dve", identb[:], identf[:], [identf], [identb])
        epst = p.sb("epst", [128, 1]); p.memset("dve", epst[:], EPS, [epst])
        mv = {}
        for nme, d_ in (("nm", nm), ("scl", scl_d), ("shl", shl_d), ("scc", scc_d), ("shc", shc_d)):
            mv[nme] = p.sb(nme, [128, 16]); p.dma(mv[nme][:], d_, writes=[mv[nme]])
        sfl = p.sb("sfl", [128, 16]); sfc = p.sb("sfc", [128, 16])
        p.stt(sfl[:], mv["scl"][:], 1.0, mv["nm"][:], ALU.add, ALU.mult, [mv["scl"], mv["nm"]], [sfl])
        p.stt(sfc[:], mv["scc"][:], 1.0, mv["nm"][:], ALU.add, ALU.mult, [mv["scc"], mv["nm"]], [sfc])
        wb = p.sb("wb", [128, 16, IN_W], BF16)
        for c4 in range(4):
            p.dma(wb[:, c4 * 4:(c4 + 1) * 4, :],
                  w_in[c4 * 512:(c4 + 1) * 512, :].rearrange("(c p) n -> p c n", p=128), writes=[wb], q="pool")
        junk = p.sb("junk", [128, D], BF16)
        ss = p.sb("ss", [128, 1]); rs = p.sb("rs", [128, 1])
        xn = [p.sb(f"xn{i}", [128, D], BF16) for i in range(2)]
        hT = [p.sb(f"hT{i}", [128, 16, 128], BF16) for i in range(2)]
        pj = [p.sb(f"pj{i}", [128, IN_W]) for i in range(2)]
        psT = [p.ps(f"psT{i}", [128, 512], BF16) for i in range(2)]
        psP = [p.ps(f"psP{i}", [128, 512]) for i in range(4)]
    for ti, (r0, np_, is_ctx) in enumerate(tok_tiles(has_ctx)):
        x_ = xt[ti % 2]
        p.dma(x_[:np_, :], xin[r0:r0 + np_, :], writes=[x_])
        if do_comb:
            g_ = gc if is_ctx else gl
            acc = yt[2]
            for k in range(N_CORES):
                y_ = yt[k % 2]
                p.dma(y_[:np_, :], yps[k][r0:r0 + np_, :], writes=[y_])
                if k == 0:
                    p.copy("pool", acc[:np_, :], y_[:np_, :], [y_], [acc])
                else:
                    p.tt("dve" if k % 2 else "pool", acc[:np_, :], acc[:np_, :], y_[:np_, :], ALU.add, [acc, y_], [acc])
            p.tt("dve", acc[:np_, :], acc[:np_, :], g_[:np_, :], ALU.mult, [acc, g_], [acc])
            p.tt("pool", x_[:np_, :], x_[:np_, :], acc[:np_, :], ALU.add, [x_, acc], [x_])
            p.dma(xout[r0:r0 + np_, :], x_[:np_, :], reads=[x_], is_output=True)
        if do_proj:
            sf = sfc if is_ctx else sfl
            sh = mv["shc"] if is_ctx else mv["shl"]
            emit_rstd(p, x_, np_, junk, ss, rs, epst, D)
            xn_ = xn[ti % 2]; h_ = hT[ti % 2]; pj_ = pj[ti % 2]
            p.ts("dve", xn_[:np_, :], x_[:np_, :], rs[:np_, :], None, ALU.mult, None, [x_, rs], [xn_])
            for g4 in range(4):
                pt = psT[g4 % 2]
                for j in range(4):
                    c = g4 * 4 + j
                    p.tr(pt[:, j * 128:j * 128 + np_], xn_[:np_, c * 128:(c + 1) * 128], identb[:np_, :np_],
                         [xn_, identb], [pt], signal=(j == 3))
                for j in range(4):
                    c = g4 * 4 + j
                    p.act(h_[:, c, :np_], pt[:, j * 128:j * 128 + np_], AF.Identity, [pt, sf, sh], [h_],
                          scale=sf[:, c:c + 1], bias=sh[:, c:c + 1])
            for nb, n0 in enumerate(range(0, IN_W, 512)):
                nn = min(512, IN_W - n0)
                pp = psP[nb]
                for c in range(16):
                    p.mm(pp[:np_, :nn], h_[:, c, :np_], wb[:, c, n0:n0 + nn], c == 0, c == 15, [h_, wb], [pp])
                p.copy("act" if nb % 2 else "dve", pj_[:np_, n0:n0 + nn], pp[:np_, :nn], [pp], [pj_])
            p.dma(proj[r0:r0 + np_, :], pj_[:np_, :], reads=[pj_], is_output=True)
    p.finish()
    return nc


def build_tok2(has_ctx=True):
    NTOK = 1088 if has_ctx else 1024
    nc = new_nc()
    xin = din(nc, "xin", [NTOK, D])
    ymT = din(nc, "ymT", [D, NTOK])
    identd = din(nc, "ident", [128, 128])
    wglu = din(nc, "w_glu", [512, 512]); bglu = din(nc, "b_glu", [128, 4])
    onrm = din(nc, "out_norm", [128, 16]); wout = din(nc, "w_out", [D, D])
    g1l = din(nc, "g1_lat", [128, D]); g1c = din(nc, "g1_ctx", [128, D])
    nm2 = din(nc, "nm2", [128, D])
    sc2l = din(nc, "sc2_lat", [128, D]); sh2l = din(nc, "sh2_lat", [128, D])
    sc2c = din(nc, "sc2_ctx", [128, D]); sh2c = din(nc, "sh2_ctx", [128, D])
    wr = din(nc, "w_router", [D, NE]); br = din(nc, "b_router", [128, NE])
    xmid = dout(nc, "xmid", [NTOK, D]); h2o = dout(nc, "h2", [NTOK, D]); gto = dout(nc, "gates", [NTOK, NE])

    p = Prog(nc)
    identf = p.sb("identf", [128, 128]); p.dma(identf[:], identd, writes=[identf])
    onesb = p.sb("onesb", [128, 128], BF16); p.memset("dve", onesb[:], 1.0, [onesb])
    epst = p.sb("epst", [128, 1]); p.memset("dve", epst[:], EPS, [epst])
    wgb = p.sb("wgb", [128, 4, 512], BF16)
    p.dma(wgb[:], wglu.rearrange("(c p) n -> p c n", p=128), writes=[wgb], q="pool")
    bgl = p.sb("bgl", [128, 4]); p.dma(bgl[:], bglu, writes=[bgl])
    on = p.sb("on", [128, 16]); p.dma(on[:], onrm, writes=[on])
    wob = p.sb("wob", [128, 16, D], BF16)
    for c4 in range(4):
        p.dma(wob[:, c4 * 4:(c4 + 1) * 4, :], wout[c4 * 512:(c4 + 1) * 512, :].rearrange("(c p) n -> p c n", p=128),
              writes=[wob], q="pool")
    wrf = p.sb("wrf", [128, 16, NE]); p.dma(wrf[:], wr.rearrange("(c p) n -> p c n", p=128), writes=[wrf])
    brt = p.sb("brt", [128, NE]); p.dma(brt[:], br, writes=[brt])
    g1 = {}; sf = {}; sh = {}
    variants = (("l", g1l, sc2l, sh2l),) + ((("c", g1c, sc2c, sh2c),) if has_ctx else ())
    nmt = p.sb("nmt", [128, D]); p.dma(nmt[:], nm2, writes=[nmt])
    for key, g_d, sc_d, sh_d in variants:
        g1[key] = p.sb(f"g1{key}", [128, D]); p.dma(g1[key][:], g_d, writes=[g1[key]])
        sf[key] = p.sb(f"sf{key}", [128, D]); p.dma(sf[key][:], sc_d, writes=[sf[key]])
        sh[key] = p.sb(f"sh{key}", [128, D]); p.dma(sh[key][:], sh_d, writes=[sh[key]])
        p.stt(sf[key][:], sf[key][:], 1.0, nmt[:], ALU.add, ALU.mult, [sf[key], nmt], [sf[key]])

    ym = [p.sb(f"ym{i}", [128, 16, 128]) for i in range(2)]
    gbf = p.sb("gbf", [128, 4, 128], BF16)
    sig = p.sb("sig", [128, 128])
    sq = p.sb("sq", [128, 16, 128], BF16)
    rsg = p.sb("rsg", [128, 3, 128])
    yn = p.sb("yn", [128, 16, 128], BF16)
    xt = [p.sb(f"xt{i}", [128, D]) for i in range(2)]
    tmp = p.sb("tmp", [128, D])
    h2t = [p.sb(f"h2t{i}", [128, D]) for i in range(2)]
    junk = p.sb("junk", [128, D], BF16)
    ss = p.sb("ss", [128, 1]); rs = p.sb("rs", [128, 1])
    h2T = p.sb("h2T", [128, 16, 128])
    lg = p.sb("lg", [128, NE]); top8 = p.sb("top8", [128, 8]); nmax = p.sb("nmax", [128, 1])
    msk = p.sb("msk", [128, NE]); ex = p.sb("ex", [128, NE]); ssum = p.sb("ssum", [128, 1])
    gt = [p.sb(f"gt{i}", [128, NE]) for i in range(2)]

    psZ = [p.ps(f"psZ{i}", [128, 512]) for i in range(2)]
    psS = p.ps("psS", [128, 3, 128])
    psO = [p.ps(f"psO{i}", [128, 512]) for i in range(4)]
    psL = p.ps("psL", [128, 512])

    groups = [(0, 4, 512.0), (4, 8, 512.0), (8, 16, 1024.0)]
    for ti, (r0, np_, is_ctx) in enumerate(tok_tiles(has_ctx)):
        key = "c" if is_ctx else "l"
        ym_ = ym[ti % 2]; x_ = xt[ti % 2]; h2_ = h2t[ti % 2]; gt_ = gt[ti % 2]
        p.dma(ym_[:, :, :np_], ymT[:, r0:r0 + np_].rearrange("(c p) n -> p c n", p=128), writes=[ym_])
        p.dma(x_[:np_, :], xin[r0:r0 + np_, :], writes=[x_])
        p.copy("act", gbf[:, :, :np_], ym_[:, 4:8, :np_], [ym_], [gbf])
        for oc in range(4):
            pz = psZ[oc % 2]
            for kc in range(4):
                p.mm(pz[:, :np_], wgb[:, kc, oc * 128:(oc + 1) * 128], gbf[:, kc, :np_], kc == 0, kc == 3,
                     [wgb, gbf], [pz])
            p.act(sig[:, :np_], pz[:, :np_], AF.Sigmoid, [pz, bgl], [sig], bias=bgl[:, oc:oc + 1])
            p.tt("dve", ym_[:, 4 + oc, :np_], ym_[:, 4 + oc, :np_], sig[:, :np_], ALU.mult, [ym_, sig], [ym_])
        p.act(sq[:, :, :np_], ym_[:, :, :np_], AF.Square, [ym_], [sq])
        for gi, (c0, c1, nf) in enumerate(groups):
            for c in range(c0, c1):
                p.mm(psS[:, gi, :np_], onesb[:], sq[:, c, :np_], c == c0, c == c1 - 1, [onesb, sq], [psS])
        for gi, (c0, c1, nf) in enumerate(groups):
            p.act(rsg[:, gi, :np_], psS[:, gi, :np_], AF.Sqrt, [psS, epst], [rsg], scale=1.0 / nf, bias=epst[:, :])
        p.op("dve", lambda e: e.reciprocal(rsg[:, :, :np_], rsg[:, :, :np_]), [rsg], [rsg])
        for gi, (c0, c1, nf) in enumerate(groups):
            for c in range(c0, c1):
                p.stt(yn[:, c, :np_], ym_[:, c, :np_], on[:, c:c + 1], rsg[:, gi, :np_], ALU.mult, ALU.mult,
                      [ym_, on, rsg], [yn])
        for nb in range(4):
            po = psO[nb]
            for c in range(16):
                p.mm(po[:np_, :], yn[:, c, :np_], wob[:, c, nb * 512:(nb + 1) * 512], c == 0, c == 15, [yn, wob], [po])
            p.tt("dve", tmp[:np_, nb * 512:(nb + 1) * 512], po[:np_, :], g1[key][:np_, nb * 512:(nb + 1) * 512],
                 ALU.mult, [po, g1[key]], [tmp])
        p.tt("pool", x_[:np_, :], x_[:np_, :], tmp[:np_, :], ALU.add, [x_, tmp], [x_])
        p.dma(xmid[r0:r0 + np_, :], x_[:np_, :], reads=[x_], is_output=True)
        emit_rstd(p, x_, np_, junk, ss, rs, epst, D)
        p.stt(tmp[:np_, :], x_[:np_, :], rs[:np_, :], sf[key][:np_, :], ALU.mult, ALU.mult, [x_, rs, sf[key]], [tmp])
        p.tt("pool", h2_[:np_, :], tmp[:np_, :], sh[key][:np_, :], ALU.add, [tmp, sh[key]], [h2_])
        p.dma(h2o[r0:r0 + np_, :], h2_[:np_, :], reads=[h2_], is_output=True)
        for g4 in range(4):
            pz = psZ[g4 % 2]
            for j in range(4):
                c = g4 * 4 + j
                p.tr(pz[:, j * 128:j * 128 + np_], h2_[:np_, c * 128:(c + 1) * 128], identf[:np_, :np_],
                     [h2_, identf], [pz])
            for j in range(4):
                c = g4 * 4 + j
                p.copy("act" if j % 2 else "dve", h2T[:, c, :np_], pz[:, j * 128:j * 128 + np_], [pz], [h2T])
        for c in range(16):
            p.mm(psL[:np_, :NE], h2T[:, c, :np_], wrf[:, c, :], c == 0, c == 15, [h2T, wrf], [psL])
        p.tt("dve", lg[:np_, :], psL[:np_, :NE], brt[:np_, :], ALU.add, [psL, brt], [lg])
        p.op("dve", lambda e: e.max(top8[:np_, :], lg[:np_, :]), [lg], [top8])
        p.ts("dve", msk[:np_, :], lg[:np_, :], top8[:np_, 3:4], None, ALU.is_ge, None, [lg, top8], [msk])
        p.ts("dve", nmax[:np_, :], top8[:np_, 0:1], -1.0, None, ALU.mult, None, [top8], [nmax])
        p.act(ex[:np_, :], lg[:np_, :], AF.Exp, [lg, nmax], [ex], bias=nmax[:np_, :])
        p.tt("dve", ex[:np_, :], ex[:np_, :], msk[:np_, :], ALU.mult, [ex, msk], [ex])
        p.op("dve", lambda e: e.reduce_sum(ssum[:np_, :], ex[:np_, :], axis=AX.X), [ex], [ssum])
        p.op("dve", lambda e: e.reciprocal(ssum[:np_, :], ssum[:np_, :]), [ssum], [ssum])
        p.ts("dve", gt_[:np_, :], ex[:np_, :], ssum[:np_, :], None, ALU.mult, None, [ex, ssum], [gt_])
        p.dma(gto[r0:r0 + np_, :], gt_[:np_, :], reads=[gt_], is_output=True)
    p.finish()
    return nc


NS = LC + L
NTS = NS // 128
TC = 512
SM_SCALE = 192 ** -0.5
TWO_PI = 2.0 * np.pi


def mix_chunks():
    return [(0, LC)] + [(LC + TC * c, TC) for c in range(L // TC)]


def build_mix():
    nc = new_nc()
    pu = din(nc, "pu", [NS, 128]); usT = din(nc, "usT", [128, NS])
    cqT = din(nc, "cqT", [512, NS]); ckvT = din(nc, "ckvT", [256, NS]); krT = din(nc, "krT", [64, NS])
    pm = din(nc, "pm", [5, 128, 128]); wpool = din(nc, "w_pool", [128, 128]); pscale = din(nc, "pscale", [128, 1])
    are_d = din(nc, "are", [128, 8]); aim_d = din(nc, "aim", [128, 8]); lst_d = din(nc, "lstep", [128, 8])
    bpr_d = din(nc, "bpad_re", [8, 128, 128]); bpi_d = din(nc, "bpad_im", [8, 128, 128])
    cpr_d = din(nc, "cpad_re", [8, 128, 128]); cpi_d = din(nc, "cpad_im", [8, 128, 128])
    dsk_d = din(nc, "dskip", [128, 1])
    qan_d = din(nc, "qan", [128, 4]); wuq_d = din(nc, "wuq", [512, 384])
    kvan_d = din(nc, "kvan", [128, 2]); wukv_d = din(nc, "wukv", [256, 512])
    qnn_d = din(nc, "qn_nope", [128, 1]); qnr_d = din(nc, "qn_rope", [64, 1])
    knn_d = din(nc, "kn_nope", [128, 1]); knr_d = din(nc, "kn_rope", [64, 1])
    cos_d = din(nc, "cos2", [64, L]); sin_d = din(nc, "sin2", [64, L]); prot_d = din(nc, "prot", [64, 64])
    identd = din(nc, "ident", [128, 128])
    ypoolT = dout(nc, "ypoolT", [128, NS]); gssmT = dout(nc, "gssmT", [128, NS]); ymla = dout(nc, "ymla", [NS, 256])

    p = Prog(nc)
    identf = p.sb("identf", [128, 128]); p.dma(identf[:], identd, writes=[identf])
    onesb = p.sb("onesb", [128, 128], BF16); p.memset("dve", onesb[:], 1.0, [onesb])
    epst = p.sb("epst", [128, 1]); p.memset("dve", epst[:], EPS, [epst])
    psg = [p.ps(f"psg{i}", [128, 512]) for i in range(8)]

    with ExitStack() as es:
        pmb = p.sbx(es, "pmb", [128, 5, 128], BF16)
        p.dma(pmb[:], pm.rearrange("k s t -> s k t"), writes=[pmb], q="pool")
        wpb = p.sbx(es, "wpb", [128, 128], BF16); p.dma(wpb[:], wpool, writes=[wpb], q="pool")
        psc = p.sbx(es, "psc", [128, 1]); p.dma(psc[:], pscale, writes=[psc])
        pub = p.sbx(es, "pub", [128, NTS, 128], BF16)
        p.dma(pub[:], pu.rearrange("(n p) c -> p n c", p=128), writes=[pub], q="pool")
        dT = p.sbx(es, "dT", [128, NS], BF16)
        yo = p.sbx(es, "yo", [128, NS])
        firsts = (0, 2); lasts = (1, NTS - 1)
        for n in range(NTS):
            ps_ = psg[(n // 4) % 2]
            o = (n % 4) * 128
            terms = []
            if n not in firsts:
                terms.append((n - 1, 0))
            terms.append((n, 3 if n in firsts else (4 if n in lasts else 1)))
            if n not in lasts:
                terms.append((n + 1, 2))
            for i, (sn, k) in enumerate(terms):
                p.mm(ps_[:, o:o + 128], pub[:, sn, :], pmb[:, k, :], i == 0, i == len(terms) - 1, [pub, pmb], [ps_])
            if n % 4 == 3 or n == NTS - 1:
                n0 = (n // 4) * 4
                w_ = (n - n0 + 1) * 128
                p.copy("act", dT[:, n0 * 128:n0 * 128 + w_], ps_[:, :w_], [ps_], [dT])
        for bi, t0 in enumerate(range(0, NS, 512)):
            n_ = min(512, NS - t0)
            ps_ = psg[2 + bi % 2]
            p.mm(ps_[:, :n_], wpb[:], dT[:, t0:t0 + n_], True, True, [wpb, dT], [ps_])
            p.act(yo[:, t0:t0 + n_], ps_[:, :n_], AF.Identity, [ps_, psc], [yo], scale=psc[:, 0:1])
        p.dma(ypoolT, yo[:], reads=[yo], is_output=True)
        p.barrier()

    with ExitStack() as es:
        sm = {}
        for nme, d_ in (("are", are_d), ("aim", aim_d), ("lst", lst_d)):
            sm[nme] = p.sbx(es, "s5" + nme, [128, 8]); p.dma(sm[nme][:], d_, writes=[sm[nme]])
        for nme in ("step", "r", "th", "kf", "m1", "s1", "c1", "abr", "abi", "den", "nr", "cre", "cim", "t0", "t1"):
            sm[nme] = p.sbx(es, "s5" + nme, [128, 8])
        ki = p.sbx(es, "s5ki", [128, 8], mybir.dt.int32)
        dsk = p.sbx(es, "dsk", [128, 1]); p.dma(dsk[:], dsk_d, writes=[dsk])
        A = lambda n: sm[n][:]
        B_ = lambda *ns: [sm[n] for n in ns]
        p.act(A("step"), A("lst"), AF.Exp, B_("lst"), B_("step"))
        p.tt("dve", A("t0"), A("are"), A("step"), ALU.mult, B_("are", "step"), B_("t0"))
        p.act(A("r"), A("t0"), AF.Exp, B_("t0"), B_("r"))
        p.tt("dve", A("th"), A("aim"), A("step"), ALU.mult, B_("aim", "step"), B_("th"))
        p.ts("dve", A("t0"), A("th"), 1.0 / TWO_PI, None, ALU.mult, None, B_("th"), B_("t0"))
        p.copy("dve", ki[:], A("t0"), B_("t0"), [ki])
        p.copy("dve", A("kf"), ki[:], [ki], B_("kf"))
        p.stt(A("th"), A("kf"), -TWO_PI, A("th"), ALU.mult, ALU.add, B_("kf", "th"), B_("th"))
        p.ts("dve", A("m1"), A("th"), float(np.pi), -TWO_PI, ALU.is_gt, ALU.mult, B_("th"), B_("m1"))
        p.tt("dve", A("th"), A("th"), A("m1"), ALU.add, B_("th", "m1"), B_("th"))
        p.ts("dve", A("m1"), A("th"), -float(np.pi), TWO_PI, ALU.is_lt, ALU.mult, B_("th"), B_("m1"))
        p.tt("dve", A("th"), A("th"), A("m1"), ALU.add, B_("th", "m1"), B_("th"))
        p.act(A("s1"), A("th"), AF.Sin, B_("th"), B_("s1"))
        p.ts("dve", A("t0"), A("th"), -1.0, None, ALU.mult, None, B_("th"), B_("t0"))
        p.tt("dve", A("t0"), A("t0"), A("th"), ALU.max, B_("t0", "th"), B_("t0"))
        p.ts("dve", A("t0"), A("t0"), -1.0, float(np.pi / 2), ALU.mult, ALU.add, B_("t0"), B_("t0"))
        p.act(A("c1"), A("t0"), AF.Sin, B_("t0"), B_("c1"))
        p.tt("dve", A("abr"), A("r"), A("c1"), ALU.mult, B_("r", "c1"), B_("abr"))
        p.tt("dve", A("abi"), A("r"), A("s1"), ALU.mult, B_("r", "s1"), B_("abi"))
        p.tt("dve", A("den"), A("are"), A("are"), ALU.mult, B_("are"), B_("den"))
        p.tt("dve", A("t0"), A("aim"), A("aim"), ALU.mult, B_("aim"), B_("t0"))
        p.tt("dve", A("den"), A("den"), A("t0"), ALU.add, B_("den", "t0"), B_("den"))
        p.op("dve", lambda e: e.reciprocal(A("den"), A("den")), B_("den"), B_("den"))
        p.ts("dve", A("nr"), A("abr"), -1.0, None, ALU.add, None, B_("abr"), B_("nr"))
        p.tt("dve", A("t0"), A("nr"), A("are"), ALU.mult, B_("nr", "are"), B_("t0"))
        p.tt("dve", A("t1"), A("abi"), A("aim"), ALU.mult, B_("abi", "aim"), B_("t1"))
        p.tt("dve", A("t0"), A("t0"), A("t1"), ALU.add, B_("t0", "t1"), B_("t0"))
        p.tt("dve", A("cre"), A("t0"), A("den"), ALU.mult, B_("t0", "den"), B_("cre"))
        p.tt("dve", A("t0"), A("abi"), A("are"), ALU.mult, B_("abi", "are"), B_("t0"))
        p.tt("dve", A("t1"), A("nr"), A("aim"), ALU.mult, B_("nr", "aim"), B_("t1"))
        p.tt("dve", A("t0"), A("t0"), A("t1"), ALU.subtract, B_("t0", "t1"), B_("t0"))
        p.tt("dve", A("cim"), A("t0"), A("den"), ALU.mult, B_("t0", "den"), B_("cim"))

        bpr = p.sbx(es, "bpr", [128, 8, 128]); bpi = p.sbx(es, "bpi", [128, 8, 128])
        p.dma(bpr[:], bpr_d.rearrange("i s c -> s i c"), writes=[bpr])
        p.dma(bpi[:], bpi_d.rearrange("i s c -> s i c"), writes=[bpi])
        BTr = p.sbx(es, "BTr", [128, 8, 128], BF16); BTi = p.sbx(es, "BTi", [128, 8, 128], BF16)
        Cr = p.sbx(es, "Cr", [128, 8, 128], BF16); Cin = p.sbx(es, "Cin", [128, 8, 128], BF16)
        p.dma(Cr[:], cpr_d.rearrange("i s c -> s i c"), writes=[Cr], q="pool")
        cif = p.sbx(es, "cif", [128, 8, 128])
        p.dma(cif[:], cpi_d.rearrange("i s c -> s i c"), writes=[cif])
        p.ts("dve", Cin[:], cif[:], -1.0, None, ALU.mult, None, [cif], [Cin])
        tb = [p.sbx(es, f"tb{i}", [128, 128]) for i in range(2)]
        for i in range(8):
            for which, (o_, a_, b__, op_) in enumerate(((BTr, bpr, bpi, ALU.subtract), (BTi, bpi, bpr, ALU.add))):
                t_ = tb[which]
                p.ts("dve", t_[:], b__[:, i, :], sm["cim"][:, i:i + 1], None, ALU.mult, None, [b__, sm["cim"]], [t_])
                p.stt(t_[:], a_[:, i, :], sm["cre"][:, i:i + 1], t_[:], ALU.mult, op_, [a_, sm["cre"], t_], [t_])
                ps_ = psg[which]
                p.tr(ps_[:, 0:128], t_[:], identf[:], [t_, identf], [ps_])
                p.copy("act", o_[:, i, :], ps_[:, 0:128], [ps_], [o_])

        cosT = p.sbx(es, "cosT", [128, 8, TC]); sinT = p.sbx(es, "sinT", [128, 8, TC])
        rt = p.sbx(es, "rt", [128, 8, TC])
        eTc = p.sbx(es, "eTc", [128, 8]); eTs = p.sbx(es, "eTs", [128, 8])
        ekc = p.sbx(es, "ekc", [128, 1]); eks = p.sbx(es, "eks", [128, 1]); ekt = p.sbx(es, "ekt", [128, 1])
        tw = [p.sbx(es, f"tw{i}", [128, TC // 2]) for i in range(2)]
        p.memset("pool", rt[:], 1.0, [rt])
        for i in range(8):
            p.ts("pool", rt[:, i, :], rt[:, i, :], sm["r"][:, i:i + 1], None, ALU.mult, None, [rt, sm["r"]], [rt])
            cT = cosT[:, i, :]; sT = sinT[:, i, :]
            p.memset("dve", cT[:, 0:1], 1.0, [cosT]); p.memset("dve", sT[:, 0:1], 0.0, [sinT])
            p.copy("dve", cT[:, 1:2], sm["c1"][:, i:i + 1], [sm["c1"]], [cosT])
            p.copy("dve", sT[:, 1:2], sm["s1"][:, i:i + 1], [sm["s1"]], [sinT])
            k = 2
            while k <= TC:
                c1_ = sm["c1"][:, i:i + 1]; s1_ = sm["s1"][:, i:i + 1]
                p.ts("dve", ekt[:], sT[:, k - 1:k], s1_, None, ALU.mult, None, [sinT, sm["s1"]], [ekt])
                p.stt(ekc[:], cT[:, k - 1:k], c1_, ekt[:], ALU.mult, ALU.subtract, [cosT, sm["c1"], ekt], [ekc])
                p.ts("dve", ekt[:], sT[:, k - 1:k], c1_, None, ALU.mult, None, [sinT, sm["c1"]], [ekt])
                p.stt(eks[:], cT[:, k - 1:k], s1_, ekt[:], ALU.mult, ALU.add, [cosT, sm["s1"], ekt], [eks])
                if k == TC:
                    p.copy("dve", eTc[:, i:i + 1], ekc[:], [ekc], [eTc])
                    p.copy("dve", eTs[:, i:i + 1], eks[:], [eks], [eTs])
                    break
                p.ts("dve", tw[0][:, :k], sT[:, 0:k], eks[:, 0:1], None, ALU.mult, None, [sinT, eks], [tw[0]])
                p.ts("dve", tw[1][:, :k], sT[:, 0:k], ekc[:, 0:1], None, ALU.mult, None, [sinT, ekc], [tw[1]])
                p.stt(sT[:, k:2 * k], cT[:, 0:k], eks[:, 0:1], tw[1][:, :k], ALU.mult, ALU.add, [cosT, eks, tw[1]], [sinT])
                p.stt(cT[:, k:2 * k], cT[:, 0:k], ekc[:, 0:1], tw[0][:, :k], ALU.mult, ALU.subtract, [cosT, ekc, tw[0]], [cosT])
                k *= 2

        uf = p.sbx(es, "uf", [128, NS]); ub = p.sbx(es, "ub", [128, NS], BF16)
        p.dma(uf[:], usT, writes=[uf])
        p.copy("act", ub[:], uf[:], [uf], [ub])
        yacc = p.sbx(es, "yacc", [128, NS])
        car = p.sbx(es, "car", [128, 8]); cai = p.sbx(es, "cai", [128, 8])
        p.memset("dve", car[:], 0.0, [car]); p.memset("dve", cai[:], 0.0, [cai])
        W = {}
        for nme in ("xr", "xi", "ta", "tb_", "tc", "td", "xtr", "xti", "gr", "gi"):
            W[nme] = [p.sbx(es, f"w_{nme}{i}", [128, TC]) for i in range(2)]
        hr = [p.sbx(es, f"hr{i}", [128, TC], BF16) for i in range(2)]
        hi = [p.sbx(es, f"hi{i}", [128, TC], BF16) for i in range(2)]
        ct = p.sbx(es, "ct", [128, 4])
        it = 0
        for d in range(2):
            chunks = mix_chunks() if d == 0 else [mix_chunks()[0]] + mix_chunks()[:0:-1]
            for ci_, (s0, n) in enumerate(chunks):
                psy = psg[4 + ci_ % 2]
                for m in range(4):
                    i = d * 4 + m
                    b2 = it % 2; it += 1
                    pxr = psg[2 * b2]; pxi = psg[2 * b2 + 1]
                    u_ap = ub[:, s0:s0 + n] if d == 0 else ub[:, s0:s0 + n][:, ::-1]
                    p.mm(pxr[:, :n], BTr[:, i, :], u_ap, True, True, [BTr, ub], [pxr])
                    p.mm(pxi[:, :n], BTi[:, i, :], u_ap, True, True, [BTi, ub], [pxi])
                    xr = W["xr"][b2]; xi = W["xi"][b2]
                    p.copy("act", xr[:, :n], pxr[:, :n], [pxr], [xr])
                    p.copy("act", xi[:, :n], pxi[:, :n], [pxi], [xi])
                    cs = cosT[:, i, :n]; sn = sinT[:, i, :n]
                    ta = W["ta"][b2]; tb2 = W["tb_"][b2]; tc_ = W["tc"][b2]; td = W["td"][b2]
                    xtr = W["xtr"][b2]; xti = W["xti"][b2]; gr = W["gr"][b2]; gi = W["gi"][b2]
                    p.tt("dve", ta[:, :n], xr[:, :n], cs, ALU.mult, [xr, cosT], [ta])
                    p.tt("pool", tb2[:, :n], xi[:, :n], sn, ALU.mult, [xi, sinT], [tb2])
                    p.tt("dve", xtr[:, :n], ta[:, :n], tb2[:, :n], ALU.add, [ta, tb2], [xtr])
                    p.tt("pool", tc_[:, :n], xi[:, :n], cs, ALU.mult, [xi, cosT], [tc_])
                    p.tt("dve", td[:, :n], xr[:, :n], sn, ALU.mult, [xr, sinT], [td])
                    p.tt("pool", xti[:, :n], tc_[:, :n], td[:, :n], ALU.subtract, [tc_, td], [xti])
                    p.op("dve", lambda e, gr=gr, xtr=xtr, i=i, n=n: e.tensor_tensor_scan(
                        gr[:, :n], rt[:, i, :n], xtr[:, :n], car[:, i:i + 1], ALU.mult, ALU.add), [rt, xtr, car], [gr])
                    p.op("dve", lambda e, gi=gi, xti=xti, i=i, n=n: e.tensor_tensor_scan(
                        gi[:, :n], rt[:, i, :n], xti[:, :n], cai[:, i:i + 1], ALU.mult, ALU.add), [rt, xti, cai], [gi])
                    if n == TC:
                        cn = eTc[:, i:i + 1]; snn = eTs[:, i:i + 1]; rd = [eTc, eTs]
                    else:
                        cn = cosT[:, i, n:n + 1]; snn = sinT[:, i, n:n + 1]; rd = [cosT, sinT]
                    p.ts("dve", ct[:, 0:1], gi[:, n - 1:n], snn, None, ALU.mult, None, [gi] + rd, [ct])
                    p.ts("dve", ct[:, 1:2], gi[:, n - 1:n], cn, None, ALU.mult, None, [gi] + rd, [ct])
                    p.stt(car[:, i:i + 1], gr[:, n - 1:n], cn, ct[:, 0:1], ALU.mult, ALU.subtract, [gr, ct] + rd, [car])
                    p.stt(cai[:, i:i + 1], gr[:, n - 1:n], snn, ct[:, 1:2], ALU.mult, ALU.add, [gr, ct] + rd, [cai])
                    hr_ = hr[b2]; hi_ = hi[b2]
                    p.tt("pool", ta[:, :n], gr[:, :n], cs, ALU.mult, [gr, cosT], [ta])
                    p.tt("dve", tb2[:, :n], gi[:, :n], sn, ALU.mult, [gi, sinT], [tb2])
                    p.tt("pool", hr_[:, :n], ta[:, :n], tb2[:, :n], ALU.subtract, [ta, tb2], [hr_])
                    p.tt("dve", tc_[:, :n], gr[:, :n], sn, ALU.mult, [gr, sinT], [tc_])
                    p.tt("pool", td[:, :n], gi[:, :n], cs, ALU.mult, [gi, cosT], [td])
                    p.tt("dve", hi_[:, :n], tc_[:, :n], td[:, :n], ALU.add, [tc_, td], [hi_])
                    hr_ap = hr_[:, :n] if d == 0 else hr_[:, :n][:, ::-1]
                    hi_ap = hi_[:, :n] if d == 0 else hi_[:, :n][:, ::-1]
                    p.mm(psy[:, :n], Cr[:, i, :], hr_ap, m == 0, False, [Cr, hr_], [psy])
                    p.mm(psy[:, :n], Cin[:, i, :], hi_ap, False, m == 3, [Cin, hi_], [psy])
                if d == 0:
                    p.copy("act", yacc[:, s0:s0 + n], psy[:, :n], [psy], [yacc])
                else:
                    p.tt("dve", yacc[:, s0:s0 + n], psy[:, :n], yacc[:, s0:s0 + n], ALU.add, [psy, yacc], [yacc])
        for t0 in range(0, NS, 1088):
            sl = slice(t0, t0 + 1088)
            z = yacc[:, sl]
            p.stt(z, uf[:, sl], dsk[:, 0:1], z, ALU.mult, ALU.add, [uf, dsk, yacc], [yacc])
            p.tt("pool", uf[:, sl], z, z, ALU.mult, [yacc], [uf])
            p.ts("dve", uf[:, sl], uf[:, sl], 0.044715, 1.0, ALU.mult, ALU.add, [uf], [uf])
            p.tt("pool", uf[:, sl], uf[:, sl], z, ALU.mult, [uf, yacc], [uf])
            p.act(uf[:, sl], uf[:, sl], AF.Sigmoid, [uf], [uf], scale=1.5957691216057308)
            p.tt("dve", z, z, uf[:, sl], ALU.mult, [yacc, uf], [yacc])
        p.dma(gssmT, yacc[:], reads=[yacc], is_output=True)
        p.barrier()

    with ExitStack() as es:
        QN = p.sbx(es, "QN", [128, 2, NS], BF16); QR = p.sbx(es, "QR", [64, 2, NS], BF16)
        KN = p.sbx(es, "KN", [128, 2, NS], BF16); KR = p.sbx(es, "KR", [64, 2, NS], BF16)
        V = p.sbx(es, "V", [128, NTS, 2, 129], BF16)
        p.memset("pool", V[:], 1.0, [V])
        wuqb = p.sbx(es, "wuqb", [128, 4, 384], BF16)
        p.dma(wuqb[:], wuq_d.rearrange("(c p) n -> p c n", p=128), writes=[wuqb], q="pool")
        wukvb = p.sbx(es, "wukvb", [128, 2, 512], BF16)
        p.dma(wukvb[:], wukv_d.rearrange("(c p) n -> p c n", p=128), writes=[wukvb], q="pool")
        protb = p.sbx(es, "protb", [64, 64], BF16); p.dma(protb[:], prot_d, writes=[protb], q="pool")
        cos2 = p.sbx(es, "cos2", [64, L]); sin2 = p.sbx(es, "sin2", [64, L])
        p.dma(cos2[:], cos_d, writes=[cos2]); p.dma(sin2[:], sin_d, writes=[sin2])
        sv = {}
        for nme, d_, shp in (("qan", qan_d, [128, 4]), ("kvan", kvan_d, [128, 2]), ("qnn", qnn_d, [128, 1]),
                             ("qnr", qnr_d, [64, 1]), ("knn", knn_d, [128, 1]), ("knr", knr_d, [64, 1])):
            sv[nme] = p.sbx(es, "m_" + nme, shp); p.dma(sv[nme][:], d_, writes=[sv[nme]])
        cq = [p.sbx(es, f"cq{i}", [128, 4, 512]) for i in range(2)]
        ckv = [p.sbx(es, f"ckv{i}", [128, 2, 512]) for i in range(2)]
        krt = [p.sbx(es, f"krt{i}", [64, 512]) for i in range(2)]
        sqb = p.sbx(es, "sqb", [128, 4, 512], BF16)
        sqkr = p.sbx(es, "sqkr", [64, 512], BF16)
        sq1 = p.sbx(es, "sq1", [128, 512], BF16); sq2 = p.sbx(es, "sq2", [64, 512], BF16)
        cqn = p.sbx(es, "cqn", [128, 4, 512], BF16); ckvn = p.sbx(es, "ckvn", [128, 2, 512], BF16)
        rq = p.sbx(es, "rq", [128, 512]); rkv = p.sbx(es, "rkv", [128, 512]); rh = p.sbx(es, "rh", [128, 512])
        rn = p.sbx(es, "rn", [64, 512], BF16); r1 = p.sbx(es, "r1", [64, 512]); r2 = p.sbx(es, "r2", [64, 512])

        def rstd_from(ps_, dst, n, nf):
            p.act(dst[:, :n], ps_[:, :n], AF.Sqrt, [ps_, epst], [dst], scale=1.0 / nf, bias=epst[:, :])
            p.op("dve", lambda e: e.reciprocal(dst[:, :n], dst[:, :n]), [dst], [dst])

        def head_norm(ps_main, sq_rope, n, nf=192.0):
            p.act(sq1[:, :n], ps_main[:, :n], AF.Square, [ps_main], [sq1])
            pn = psg[5]
            p.mm(pn[:, :n], onesb[:, :], sq1[:, :n], True, False, [onesb, sq1], [pn])
            p.mm(pn[:, :n], onesb[0:64, :], sq_rope[0:64, :n], False, True, [onesb, sq_rope], [pn])
            rstd_from(pn, rh, n, nf)

        def rope_store(dst, hl, t0, n, is_ctx):
            if is_ctx:
                p.copy("pool", dst[0:64, hl, t0:t0 + n], rn[0:64, :n], [rn], [dst])
                return
            pp = psg[6]
            p.mm(pp[0:64, :n], protb[0:64, 0:64], rn[0:64, :n], True, True, [protb, rn], [pp])
            q0 = t0 - LC
            p.tt("pool", r1[:, :n], rn[0:64, :n], cos2[:, q0:q0 + n], ALU.mult, [rn, cos2], [r1])
            p.tt("dve", r2[:, :n], pp[0:64, :n], sin2[:, q0:q0 + n], ALU.mult, [pp, sin2], [r2])
            p.tt("dve", dst[0:64, hl, t0:t0 + n], r1[:, :n], r2[:, :n], ALU.add, [r1, r2], [dst])

        for bi, (t0, n) in enumerate(mix_chunks()):
            is_ctx = bi == 0
            cq_ = cq[bi % 2]; ckv_ = ckv[bi % 2]; kr_ = krt[bi % 2]
            p.dma(cq_[:, :, :n], cqT[:, t0:t0 + n].rearrange("(c p) n -> p c n", p=128), writes=[cq_])
            p.dma(ckv_[:, :, :n], ckvT[:, t0:t0 + n].rearrange("(c p) n -> p c n", p=128), writes=[ckv_])
            p.dma(kr_[:, :n], krT[:, t0:t0 + n], writes=[kr_])
            p.act(sqb[:, :, :n], cq_[:, :, :n], AF.Square, [cq_], [sqb])
            pa = psg[0]
            for c in range(4):
                p.mm(pa[:, :n], onesb[:], sqb[:, c, :n], c == 0, c == 3, [onesb, sqb], [pa])
            rstd_from(pa, rq, n, 512.0)
            for c in range(4):
                p.stt(cqn[:, c, :n], cq_[:, c, :n], sv["qan"][:, c:c + 1], rq[:, :n], ALU.mult, ALU.mult,
                      [cq_, sv["qan"], rq], [cqn])
            p.act(sqb[:, 0:2, :n], ckv_[:, :, :n], AF.Square, [ckv_], [sqb])
            pb = psg[1]
            for c in range(2):
                p.mm(pb[:, :n], onesb[:], sqb[:, c, :n], c == 0, c == 1, [onesb, sqb], [pb])
            rstd_from(pb, rkv, n, 256.0)
            for c in range(2):
                p.stt(ckvn[:, c, :n], ckv_[:, c, :n], sv["kvan"][:, c:c + 1], rkv[:, :n], ALU.mult, ALU.mult,
                      [ckv_, sv["kvan"], rkv], [ckvn])
            p.act(sqkr[:, :n], kr_[:, :n], AF.Square, [kr_], [sqkr])
            for hl in range(2):
                pq = psg[2]; pr = psg[3]
                for c in range(4):
                    p.mm(pq[:, :n], wuqb[:, c, hl * 192:hl * 192 + 128], cqn[:, c, :n], c == 0, c == 3, [wuqb, cqn], [pq])
                for c in range(4):
                    p.mm(pr[0:64, :n], wuqb[:, c, hl * 192 + 128:hl * 192 + 192], cqn[:, c, :n], c == 0, c == 3,
                         [wuqb, cqn], [pr])
                p.act(sq2[:, :n], pr[0:64, :n], AF.Square, [pr], [sq2])
                head_norm(pq, sq2, n)
                p.stt(QN[:, hl, t0:t0 + n], pq[:, :n], sv["qnn"][:, 0:1], rh[:, :n], ALU.mult, ALU.mult,
                      [pq, sv["qnn"], rh], [QN])
                p.stt(rn[0:64, :n], pr[0:64, :n], sv["qnr"][:, 0:1], rh[0:64, :n], ALU.mult, ALU.mult,
                      [pr, sv["qnr"], rh], [rn])
                rope_store(QR, hl, t0, n, is_ctx)
                pk = psg[4]
                for c in range(2):
                    p.mm(pk[:, :n], wukvb[:, c, hl * 256:hl * 256 + 128], ckvn[:, c, :n], c == 0, c == 1, [wukvb, ckvn], [pk])
                head_norm(pk, sqkr, n)
                p.stt(KN[:, hl, t0:t0 + n], pk[:, :n], sv["knn"][:, 0:1], rh[:, :n], ALU.mult, ALU.mult,
                      [pk, sv["knn"], rh], [KN])
                p.stt(rn[0:64, :n], kr_[:, :n], sv["knr"][:, 0:1], rh[0:64, :n], ALU.mult, ALU.mult,
                      [kr_, sv["knr"], rh], [rn])
                rope_store(KR, hl, t0, n, is_ctx)
                for tt in range(n // 128):
                    pv = psg[7]
                    for c in range(2):
                        p.mm(pv[:, 0:128], ckvn[:, c, tt * 128:(tt + 1) * 128], wukvb[:, c, hl * 256 + 128:hl * 256 + 256],
                             c == 0, c == 1, [ckvn, wukvb], [pv])
                    p.copy("act", V[:, t0 // 128 + tt, hl, 0:128], pv[:, 0:128], [pv], [V])

        PT = [p.sbx(es, f"PT{i}", [128, 512], BF16) for i in range(3)]
        osb = [p.sbx(es, f"osb{i}", [128, 4, 128]) for i in range(2)]
        rec = p.sbx(es, "rec", [128, 4])
        qblocks = [(0, LC, list(range(LC // 128)))] + [(LC + 512 * qb, 512, list(range(NTS))) for qb in range(L // 512)]
        it = 0
        for hl in range(2):
            for qi, (q0, qn_, kts) in enumerate(qblocks):
                nq = qn_ // 128
                for kt in kts:
                    pss = psg[it % 2]; pt_ = PT[it % 3]; it += 1
                    p.mm(pss[:, :qn_], KN[:, hl, kt * 128:(kt + 1) * 128], QN[:, hl, q0:q0 + qn_], True, False, [KN, QN], [pss])
                    p.mm(pss[:, :qn_], KR[0:64, hl, kt * 128:(kt + 1) * 128], QR[0:64, hl, q0:q0 + qn_], False, True,
                         [KR, QR], [pss])
                    p.act(pt_[:, :qn_], pss[:, :qn_], AF.Exp, [pss], [pt_], scale=SM_SCALE)
                    for qs in range(nq):
                        p.mm(psg[2 + qs][:, 0:129], pt_[:, qs * 128:(qs + 1) * 128], V[:, kt, hl, :],
                             kt == kts[0], kt == kts[-1], [pt_, V], [psg[2 + qs]])
                o_ = osb[qi % 2]
                for qs in range(nq):
                    p.op("dve", lambda e, qs=qs: e.reciprocal(rec[:, qs:qs + 1], psg[2 + qs][:, 128:129]), [psg[2 + qs]], [rec])
                    p.ts("dve", o_[:, qs, :], psg[2 + qs][:, 0:128], rec[:, qs:qs + 1], None, ALU.mult, None,
                         [psg[2 + qs], rec], [o_])
                p.dma(ymla[q0:q0 + qn_, hl * 128:(hl + 1) * 128].rearrange("(s p) d -> p s d", p=128), o_[:, :nq, :],
                      reads=[o_], is_output=True)
        p.barrier()
    p.finish()
    return nc


POOL_WINDOWS = (2, 4, 8, 16)


def pool_matrices(w):
    n = 384
    P = np.zeros((n, n), np.float64)
    for t in range(n):
        lo = max(t - w // 2, 0); hi = min(t + (w - w // 2), n)
        P[t, lo:hi] = 1.0 / (hi - lo)
        P[t, t] -= 1.0
    R = P.T
    blk = lambda si, ti: R[si * 128:(si + 1) * 128, ti * 128:(ti + 1) * 128]
    return np.stack([blk(0, 1), blk(1, 1), blk(2, 1), blk(0, 0), blk(2, 2)]).astype(np.float32)


def rope_tables():
    rows = L // 64
    row = np.repeat(np.arange(rows), 64).astype(np.float32)
    col = np.tile(np.arange(64), rows).astype(np.float32)
    inv = (10000.0 ** (-np.arange(0, 32, 2, dtype=np.float32) / 32)).astype(np.float32)
    ang = np.concatenate([row[:, None] * inv, col[:, None] * inv], axis=-1).astype(np.float32)
    cos, sin = np.cos(ang).astype(np.float32), np.sin(ang).astype(np.float32)
    cos2 = np.ascontiguousarray(np.concatenate([cos, cos], 1).T)
    sin2 = np.ascontiguousarray(np.concatenate([sin, sin], 1).T)
    prot = np.zeros((64, 64), np.float32)
    for m in range(32):
        prot[m + 32, m] = -1.0
        prot[m, m + 32] = 1.0
    return cos2, sin2, prot


def pl(v):
    return np.ascontiguousarray(np.asarray(v).reshape(-1, 128).T)


def bc(v):
    return np.ascontiguousarray(np.broadcast_to(np.asarray(v)[None, :], (128, v.shape[0])))


def prep_mix(inp, l, j, proj_b, consts):
    ident, cos2, sin2, prot = consts
    m = {}
    m["pu"] = np.ascontiguousarray(proj_b[:, 128 * j:128 * j + 128])
    m["usT"] = np.ascontiguousarray(proj_b[:, OFF_SSM + 128 * j:OFF_SSM + 128 * j + 128].T)
    m["cqT"] = np.ascontiguousarray(proj_b[:, OFF_Q:OFF_Q + 512].T)
    m["ckvT"] = np.ascontiguousarray(proj_b[:, OFF_KV:OFF_KV + 256].T)
    m["krT"] = np.ascontiguousarray(proj_b[:, OFF_KR:OFF_KR + 64].T)
    m["pm"] = pool_matrices(POOL_WINDOWS[j])
    m["w_pool"] = np.ascontiguousarray(inp["w_pool"][l, j])
    m["pscale"] = np.ascontiguousarray(inp["pool_scale"][l, 128 * j:128 * j + 128][:, None])
    are = np.zeros((128, 8), np.float32); aim = np.zeros((128, 8), np.float32); lst = np.zeros((128, 8), np.float32)
    bpr = np.zeros((8, 128, 128), np.float32); bpi = np.zeros((8, 128, 128), np.float32)
    cpr = np.zeros((8, 128, 128), np.float32); cpi = np.zeros((8, 128, 128), np.float32)
    for d in range(2):
        for mm_ in range(4):
            i = d * 4 + mm_
            for h in range(2):
                gl = 2 * mm_ + h
                g = 8 * j + gl
                ps_ = slice(64 * h, 64 * h + 64); cs_ = slice(16 * gl, 16 * gl + 16)
                are[ps_, i] = inp["ssm_a_re"][l, d, g]; aim[ps_, i] = inp["ssm_a_im"][l, d, g]
                lst[ps_, i] = inp["ssm_log_step"][l, d, g]
                bpr[i, ps_, cs_] = inp["ssm_b_re"][l, d, g]; bpi[i, ps_, cs_] = inp["ssm_b_im"][l, d, g]
                cpr[i, ps_, cs_] = inp["ssm_c_re"][l, d, g].T; cpi[i, ps_, cs_] = inp["ssm_c_im"][l, d, g].T
    m.update({"are": are, "aim": aim, "lstep": lst, "bpad_re": bpr, "bpad_im": bpi, "cpad_re": cpr, "cpad_im": cpi})
    m["dskip"] = np.ascontiguousarray(inp["ssm_d"][l, 128 * j:128 * j + 128][:, None])
    m["qan"] = pl(inp["q_a_norm"][l]); m["kvan"] = pl(inp["kv_a_norm"][l])
    m["wuq"] = np.ascontiguousarray(inp["w_uq"][l][:, 384 * j:384 * j + 384])
    m["wukv"] = np.ascontiguousarray(inp["w_ukv"][l][:, 512 * j:512 * j + 512])
    qn = inp["q_norm"][l]; kn = inp["k_norm"][l]
    m["qn_nope"] = np.ascontiguousarray(qn[:128, None]); m["qn_rope"] = np.ascontiguousarray(qn[128:, None])
    m["kn_nope"] = np.ascontiguousarray(kn[:128, None]); m["kn_rope"] = np.ascontiguousarray(kn[128:, None])
    m["cos2"] = cos2; m["sin2"] = sin2; m["prot"] = prot; m["ident"] = ident
    return m


_PROGS = {}


def _prog(key, builder):
    if key not in _PROGS:
        _PROGS[key] = builder()
    return _PROGS[key]


def _run(nc, maps):
    res = run_bass_kernel_spmd(nc, maps, core_ids=list(range(len(maps))))
    return res.results


def _c(a):
    return np.ascontiguousarray(a, dtype=np.float32)


def kernel(**inputs):
    inp = {k: np.asarray(v, dtype=np.float32) for k, v in inputs.items()}
    ident, ltri = host_consts()
    cos2, sin2, prot = rope_tables()
    mconsts = (ident, cos2, sin2, prot)

    cv = np.stack([inp["c"][0], inp["c"][1], inp["c_ctx"]], 0)
    cvT = _c(cv.reshape(3, 16, 128).transpose(2, 1, 0))
    maps = []
    for k in range(N_CORES):
        cs = slice(k * ADA_COLS, (k + 1) * ADA_COLS)
        maps.append({"cvT": cvT, "wa": _c(inp["w_ada"][:, :, cs]), "ba": _c(inp["b_ada"][:, cs])})
    r = _run(_prog("ada", build_ada), maps)
    mods = np.concatenate([x["mods"] for x in r], axis=2)

    xrows = []
    for k in range(N_CORES):
        b, j = k // 4, k % 4
        xrows.append(_c(np.concatenate([inp["ctx"][b, 64 * j:64 * j + 64], inp["x"][b, 1024 * j:1024 * j + 1024]], 0)))
    yparts = None
    for l in range(DEPTH):
        has_ctx = l < DEPTH - 1
        mb = [mods[l, b].reshape(6, D) for b in range(B)]
        mc = mods[l, 2].reshape(6, D)
        maps = []
        for k in range(N_CORES):
            b, j = k // 4, k % 4
            m = {"xin": xrows[k], "ident": ident, "nm": pl(inp["norm_mix"][l]),
                 "sc_lat": pl(mb[b][1]), "sh_lat": pl(mb[b][0]), "sc_ctx": pl(mc[1]), "sh_ctx": pl(mc[0]),
                 "w_in": _c(inp["w_in"][l])}
            if l > 0:
                pm_ = [mods[l - 1, b_].reshape(6, D) for b_ in range(B)]
                pc_ = mods[l - 1, 2].reshape(6, D)
                for kk in range(N_CORES):
                    m[f"yp{kk}"] = _c(yparts[kk][k * 1088:(k + 1) * 1088])
                m["gb_lat"] = bc(pm_[b][5]); m["gb_ctx"] = bc(pc_[5])
            maps.append(m)
        r = _run(_prog(("tok1", l > 0), lambda: build_tok1(l > 0, True, True)), maps)
        if l > 0:
            xrows = [x["xout"] for x in r]
        projs = [x["proj"] for x in r]
        maps = []
        for k in range(N_CORES):
            b, j = k // 4, k % 4
            proj_b = np.concatenate([projs[4 * b + jj][:64] for jj in range(4)] +
                                    [projs[4 * b + jj][64:] for jj in range(4)], 0)
            maps.append(prep_mix(inp, l, j, proj_b, mconsts))
        r = _run(_prog("mix", build_mix), maps)
        ymT_b = []
        for b in range(B):
            rb = r[4 * b:4 * b + 4]
            ymT_b.append(np.concatenate([x["ypoolT"] for x in rb] + [x["gssmT"] for x in rb] +
                                        [x["ymla"].T for x in rb], 0))
        maps = []
        for k in range(N_CORES):
            b, j = k // 4, k % 4
            cols = ymT_b[b][:, LC + 1024 * j:LC + 1024 * j + 1024]
            xin = xrows[k]
            if has_ctx:
                cols = np.concatenate([ymT_b[b][:, 64 * j:64 * j + 64], cols], 1)
            else:
                xin = xin[64:]
            maps.append({"xin": _c(xin), "ymT": _c(cols), "ident": ident, "w_glu": _c(inp["w_glu"][l]),
                         "b_glu": pl(inp["b_glu"][l]), "out_norm": pl(inp["out_norm"][l]), "w_out": _c(inp["w_out"][l]),
                         "g1_lat": bc(mb[b][2]), "g1_ctx": bc(mc[2]), "nm2": bc(inp["norm_ffn"][l]),
                         "sc2_lat": bc(mb[b][4]), "sh2_lat": bc(mb[b][3]), "sc2_ctx": bc(mc[4]), "sh2_ctx": bc(mc[3]),
                         "w_router": _c(inp["w_router"][l]), "b_router": bc(inp["b_router"][l])})
        r = _run(_prog(("tok2", has_ctx), lambda: build_tok2(has_ctx)), maps)
        xmids = [x["xmid"] for x in r]
        h2_all = _c(np.concatenate([x["h2"] for x in r], 0))
        gates_all = np.concatenate([x["gates"] for x in r], 0)
        T = h2_all.shape[0]
        maps = []
        for k in range(N_CORES):
            es_ = slice(E_LOC * k, E_LOC * k + E_LOC)
            maps.append({"h2": h2_all, "gate": _c(gates_all[:, es_]),
                         "wg": _c(inp["w_gate"][l, es_]), "wu": _c(inp["w_up"][l, es_]), "wd": _c(inp["w_down"][l, es_]),
                         "bg": _c(inp["b_gate"][l, es_]), "bu": _c(inp["b_up"][l, es_]), "bd": _c(inp["b_down"][l, es_]),
                         "ident": ident, "ltri": ltri})
        r = _run(_prog(("moe", T), lambda: build_moe(T)), maps)
        yparts = [x["ypart"] for x in r]
        if has_ctx:
            xrows = xmids
        else:
            xrows = xmids
    l = DEPTH - 1
    mb = [mods[l, b].reshape(6, D) for b in range(B)]
    maps = []
    for k in range(N_CORES):
        b = k // 4
        m = {"xin": _c(xrows[k]), "gb_lat": bc(mb[b][5]), "gb_ctx": bc(mb[b][5])}
        for kk in range(N_CORES):
            m[f"yp{kk}"] = _c(yparts[kk][k * 1024:(k + 1) * 1024])
        maps.append(m)
    r = _run(_prog("final", lambda: build_tok1(True, False, False)), maps)
    out = np.zeros((B, L, D), np.float32)
    for k in range(N_CORES):
        b, j = k // 4, k % 4
        out[b, 1024 * j:1024 * j + 1024] = r[k]["xout"]
    return out
```
